# Optimizing a Trainium2 kernel written in Bass

```python
import math
import jax, jax.numpy as jnp
from jax import lax
import numpy as np

D_MODEL = 1024
BATCH = 4
SEQ = 4096
DEPTH = 4

GRID_W = 64
N_Q_HEADS = 8
N_KV_HEADS = 2
HEAD_DIM = 64
Q_BLOCK = 128
ROPE_THETA = 10000.0
ATTN_WIDTH = N_Q_HEADS * HEAD_DIM
KV_WIDTH = N_KV_HEADS * HEAD_DIM
SGU_GROUPS = 8
SGU_WIDTH = 512
SGU_GROUP_DIM = SGU_WIDTH // SGU_GROUPS
SGU_CHUNK = 128
Q_END = ATTN_WIDTH
K_END = Q_END + KV_WIDTH
V_END = K_END + KV_WIDTH
Z_END = V_END + 2 * SGU_WIDTH
GA_END = Z_END + D_MODEL
IN_COLS = GA_END + D_MODEL
N_EXPERTS = 64
TOP_K = 8
N_GROUPS = 8
TOPK_GROUPS = 4
EXPERT_FF = 256
SHARED_FF = 256
ROUTED_SCALE = 2.5
MOE_BLOCK = 128
ALPHA = (2 * DEPTH) ** 0.25
BETA = (8 * DEPTH) ** -0.25

kernel_name = 'hybrid_gqa_sgu_moe_deepnorm_encoder'


def layer_norm(x, g, b, eps=1e-5):
    xf = x.astype(jnp.float32)
    mu = jnp.mean(xf, -1, keepdims=True)
    var = jnp.mean(jnp.square(xf - mu), -1, keepdims=True)
    return ((xf - mu) * lax.rsqrt(var + eps)).astype(x.dtype) * g + b


def rms_norm(x, g, eps=1e-6):
    xf = x.astype(jnp.float32)
    return (xf * lax.rsqrt(jnp.mean(jnp.square(xf), -1, keepdims=True) + eps)).astype(x.dtype) * g


def axial_rope_tables(seq_len, rows):
    t = jnp.arange(seq_len)
    row = (t // GRID_W - rows // 2).astype(jnp.float32)
    col = (t % GRID_W - GRID_W // 2).astype(jnp.float32)
    n_pairs = HEAD_DIM // 4
    inv_freq = ROPE_THETA ** (-jnp.arange(n_pairs, dtype=jnp.float32) / n_pairs)
    ang = jnp.concatenate([row[:, None] * inv_freq, col[:, None] * inv_freq], -1)
    return jnp.cos(ang), jnp.sin(ang)


def apply_rope(x, cos, sin):
    xf = x.astype(jnp.float32).reshape(*x.shape[:-1], HEAD_DIM // 2, 2)
    x0, x1 = xf[..., 0], xf[..., 1]
    c = cos[None, :, None, :]
    s = sin[None, :, None, :]
    out = jnp.stack([x0 * c - x1 * s, x0 * s + x1 * c], -1)
    return out.reshape(x.shape).astype(x.dtype)


def attention_branch(q, k, v, q_scale, k_scale, cos, sin):
    B, S = q.shape[:2]
    grp = N_Q_HEADS // N_KV_HEADS
    q = apply_rope(rms_norm(q, q_scale), cos, sin)
    k = apply_rope(rms_norm(k, k_scale), cos, sin)
    nqb = S // Q_BLOCK
    qb = q.reshape(B, nqb, Q_BLOCK, N_KV_HEADS, grp, HEAD_DIM).transpose(1, 0, 3, 4, 2, 5)
    kt = k.transpose(0, 2, 1, 3)
    vt = v.transpose(0, 2, 1, 3)
    scale = HEAD_DIM ** -0.5

    def attend(q_blk):
        s = jnp.einsum('bkgqd,bksd->bkgqs', q_blk, kt).astype(jnp.float32) * scale
        p = jax.nn.softmax(s, axis=-1).astype(vt.dtype)
        return jnp.einsum('bkgqs,bksd->bkgqd', p, vt)

    o = lax.map(attend, qb)
    return o.transpose(1, 0, 4, 2, 3, 5).reshape(B, S, ATTN_WIDTH)


def spatial_gating_branch(z, ln_g, ln_b, w_s, b_s):
    B, S, _ = z.shape
    z = jax.nn.gelu(z, approximate=False)
    u, v = z[..., :SGU_WIDTH], z[..., SGU_WIDTH:]
    v = layer_norm(v, ln_g, ln_b)
    vc = v.reshape(B, S // SGU_CHUNK, SGU_CHUNK, SGU_GROUPS, SGU_GROUP_DIM)
    sv = jnp.einsum('gij,bnjgd->bnigd', w_s, vc) + b_s.T[:, :, None]
    return u * sv.reshape(B, S, SGU_WIDTH)


def token_mixer(h, w_in, q_scale, k_scale, sgu_ln_g, sgu_ln_b, w_s, b_s, w_branch_a, w_branch_b, w_out, cos, sin):
    B, S, _ = h.shape
    p = h @ w_in
    q = p[..., :Q_END].reshape(B, S, N_Q_HEADS, HEAD_DIM)
    k = p[..., Q_END:K_END].reshape(B, S, N_KV_HEADS, HEAD_DIM)
    v = p[..., K_END:V_END].reshape(B, S, N_KV_HEADS, HEAD_DIM)
    z = p[..., V_END:Z_END]
    gate_a = p[..., Z_END:GA_END]
    gate_b = p[..., GA_END:]
    o_a = attention_branch(q, k, v, q_scale, k_scale, cos, sin)
    o_b = spatial_gating_branch(z, sgu_ln_g, sgu_ln_b, w_s, b_s)
    y = jax.nn.sigmoid(gate_a) * (o_a @ w_branch_a) + jax.nn.sigmoid(gate_b) * (o_b @ w_branch_b)
    return y @ w_out


def moe_ffn(h, router_w, router_bias, w_gate, w_up, w_down, ws_gate, ws_up, ws_down):
    B, S, D = h.shape
    T = B * S
    xt = h.reshape(T, D)
    scores = jax.nn.sigmoid((xt @ router_w).astype(jnp.float32))
    sel = scores + router_bias.astype(jnp.float32)
    per_group = N_EXPERTS // N_GROUPS
    group_score = lax.top_k(sel.reshape(T, N_GROUPS, per_group), 2)[0].sum(-1)
    _, gidx = lax.top_k(group_score, TOPK_GROUPS)
    gmask = jnp.any(gidx[..., None] == jnp.arange(N_GROUPS), axis=-2)
    emask = jnp.repeat(gmask, per_group, axis=-1)
    _, eidx = lax.top_k(jnp.where(emask, sel, -jnp.inf), TOP_K)
    wts = jnp.take_along_axis(scores, eidx, -1)
    wts = wts / jnp.sum(wts, -1, keepdims=True) * ROUTED_SCALE
    A = T * TOP_K
    e_flat = eidx.reshape(A)
    t_flat = jnp.repeat(jnp.arange(T, dtype=jnp.int32), TOP_K)
    w_flat = wts.reshape(A)
    order = jnp.argsort(e_flat)
    e_s, t_s, w_s = e_flat[order], t_flat[order], w_flat[order]
    counts = jnp.bincount(e_flat, length=N_EXPERTS)
    starts = jnp.cumsum(counts) - counts
    padded = (counts + MOE_BLOCK - 1) // MOE_BLOCK * MOE_BLOCK
    pends = jnp.cumsum(padded)
    pstarts = pends - padded
    dest = pstarts[e_s] + (jnp.arange(A) - starts[e_s])
    n_blocks = A // MOE_BLOCK + N_EXPERTS
    P = n_blocks * MOE_BLOCK
    tok_buf = jnp.full((P,), T, jnp.int32).at[dest].set(t_s.astype(jnp.int32))
    w_buf = jnp.zeros((P,), xt.dtype).at[dest].set(w_s.astype(xt.dtype))
    blk_expert = jnp.minimum(jnp.searchsorted(pends, jnp.arange(n_blocks) * MOE_BLOCK, side='right'), N_EXPERTS - 1)
    x_pad = jnp.concatenate([xt, jnp.zeros((1, D), xt.dtype)], 0)

    def run_block(args):
        e, toks, ws = args
        xb = x_pad[toks]
        hb = jax.nn.silu(xb @ w_gate[e]) * (xb @ w_up[e])
        return (hb @ w_down[e]) * ws[:, None]

    yb = lax.map(run_block, (blk_expert, tok_buf.reshape(n_blocks, MOE_BLOCK), w_buf.reshape(n_blocks, MOE_BLOCK)))
    routed = jax.ops.segment_sum(yb.reshape(P, D), tok_buf, num_segments=T + 1)[:T]
    shared = (jax.nn.silu(xt @ ws_gate) * (xt @ ws_up)) @ ws_down
    return (routed + shared).reshape(B, S, D)


def setup_inputs(seed: int = 0) -> dict:
    key = jax.random.key(seed)
    ks = jax.random.split(key, 26)
    L, D, E = DEPTH, D_MODEL, N_EXPERTS
    f32 = jnp.float32

    def nrm(k, shape, s):
        return jax.random.normal(k, shape, f32) * s

    def gain(k, shape):
        return 1.0 + 0.02 * jax.random.normal(k, shape, f32)

    w_in = nrm(ks[4], (L, D, IN_COLS), D ** -0.5)
    w_in = w_in.at[:, :, K_END:V_END].multiply(BETA)
    return {
        'x': nrm(ks[0], (BATCH, SEQ, D), 1.0),
        'c': nrm(ks[1], (BATCH, D), 1.0),
        'w_ada': nrm(ks[2], (L, D, 6 * D), 0.5 * D ** -0.5),
        'b_ada': nrm(ks[3], (L, 6 * D), 0.02),
        'w_in': w_in,
        'q_scale': gain(ks[5], (L, HEAD_DIM)),
        'k_scale': gain(ks[6], (L, HEAD_DIM)),
        'sgu_ln_g': gain(ks[7], (L, SGU_WIDTH)),
        'sgu_ln_b': nrm(ks[8], (L, SGU_WIDTH), 0.02),
        'w_spatial': nrm(ks[9], (L, SGU_GROUPS, SGU_CHUNK, SGU_CHUNK), SGU_CHUNK ** -0.5),
        'b_spatial': nrm(ks[10], (L, SGU_GROUPS, SGU_CHUNK), 0.02),
        'w_branch_a': nrm(ks[11], (L, ATTN_WIDTH, D), ATTN_WIDTH ** -0.5),
        'w_branch_b': nrm(ks[12], (L, SGU_WIDTH, D), SGU_WIDTH ** -0.5),
        'w_out': nrm(ks[13], (L, D, D), BETA * D ** -0.5),
        'ln1_g': gain(ks[14], (L, D)),
        'ln1_b': nrm(ks[15], (L, D), 0.02),
        'router_w': nrm(ks[16], (L, D, E), D ** -0.5),
        'router_bias': nrm(ks[17], (L, E), 0.01),
        'w_gate': nrm(ks[18], (L, E, D, EXPERT_FF), D ** -0.5),
        'w_up': nrm(ks[19], (L, E, D, EXPERT_FF), D ** -0.5),
        'w_down': nrm(ks[20], (L, E, EXPERT_FF, D), BETA * EXPERT_FF ** -0.5),
        'ws_gate': nrm(ks[21], (L, D, SHARED_FF), D ** -0.5),
        'ws_up': nrm(ks[22], (L, D, SHARED_FF), D ** -0.5),
        'ws_down': nrm(ks[23], (L, SHARED_FF, D), BETA * SHARED_FF ** -0.5),
        'ln2_g': gain(ks[24], (L, D)),
        'ln2_b': nrm(ks[25], (L, D), 0.02),
    }


def reference(x, c, w_ada, b_ada, w_in, q_scale, k_scale, sgu_ln_g, sgu_ln_b, w_spatial, b_spatial,
              w_branch_a, w_branch_b, w_out, ln1_g, ln1_b, router_w, router_bias, w_gate, w_up, w_down,
              ws_gate, ws_up, ws_down, ln2_g, ln2_b):
    B, S, D = x.shape
    rows = S // GRID_W
    cos, sin = axial_rope_tables(S, rows)
    cond = jax.nn.silu(c)
    for l in range(DEPTH):
        mod = cond @ w_ada[l] + b_ada[l]
        shift1, scale1, gate1, shift2, scale2, gate2 = jnp.split(mod[:, None, :], 6, axis=-1)
        h = x * (1 + scale1) + shift1
        y = token_mixer(h, w_in[l], q_scale[l], k_scale[l], sgu_ln_g[l], sgu_ln_b[l], w_spatial[l], b_spatial[l],
                        w_branch_a[l], w_branch_b[l], w_out[l], cos, sin)
        x = layer_norm(ALPHA * x + gate1 * y, ln1_g[l], ln1_b[l])
        h = x * (1 + scale2) + shift2
        y = moe_ffn(h, router_w[l], router_bias[l], w_gate[l], w_up[l], w_down[l], ws_gate[l], ws_up[l], ws_down[l])
        x = layer_norm(ALPHA * x + gate2 * y, ln2_g[l], ln2_b[l])
    return x
```

```python
import contextlib
import os
import numpy as np
import ml_dtypes
import concourse.bass as bass
import concourse.mybir as mybir
from concourse.bass_utils import run_bass_kernel_spmd

F32 = mybir.dt.float32
BF16 = mybir.dt.bfloat16
AF = mybir.ActivationFunctionType
ALU = mybir.AluOpType
AX = mybir.AxisListType

D = 1024
SEQ = 4096
TOK = 2048
NT = TOK // 128
NBLK = 4
DEPTH = 4
ALPHA = (2 * DEPTH) ** 0.25
NEXP = 64
ENGS = ("pe", "act", "dve", "pool", "sp")
SEM_LIMIT = 15000
FUSE_WAITS = False


class Op:
    __slots__ = ("eng", "fn", "deps", "users", "sem", "val", "dma", "waits", "inc")

    def __init__(self, eng, fn, dma):
        self.eng = eng
        self.fn = fn
        self.deps = []
        self.users = 0
        self.sem = None
        self.val = 0
        self.dma = dma
        self.waits = []


class Prog:
    def __init__(self, nc):
        self.nc = nc
        self.ops = {e: [] for e in ENGS}
        self.order = []
        self.lastw = {}
        self.readers = {}
        self.last_dma = {}
        self.pending = {}

    def barrier_list(self):
        lasts = [self.ops[e][-1] for e in ENGS if self.ops[e]]
        lasts.extend(self.last_dma.values())
        return lasts

    def barrier(self):
        lasts = self.barrier_list()
        self.pending = {e: list(lasts) for e in ENGS}
        self.lastw.clear()
        self.readers.clear()

    def add(self, eng, fn, reads=(), writes=(), dma=None, inc=None):
        op = Op(eng, fn, dma)
        op.inc = inc if inc is not None else (16 if dma is not None else 1)
        deps = list(self.pending.pop(eng, ()))
        for k in reads:
            w = self.lastw.get(k)
            if w is not None:
                deps.append(w)
        for k in writes:
            w = self.lastw.get(k)
            if w is not None:
                deps.append(w)
            deps.extend(self.readers.get(k, ()))
        seen = set()
        for d in deps:
            if id(d) in seen or d is op:
                continue
            seen.add(id(d))
            if d.eng == "pe" and eng == "pe" and d.dma is None and dma is None:
                continue
            op.deps.append(d)
            d.users += 1
        for k in reads:
            self.readers.setdefault(k, []).append(op)
        for k in writes:
            self.lastw[k] = op
            self.readers[k] = []
        self.ops[eng].append(op)
        self.order.append(op)
        if dma is not None:
            self.last_dma[dma] = op
        return op

    def pe(self, fn, reads=(), writes=()):
        return self.add("pe", fn, reads, writes)

    def act(self, fn, reads=(), writes=()):
        return self.add("act", fn, reads, writes)

    def dve(self, fn, reads=(), writes=()):
        return self.add("dve", fn, reads, writes)

    def pool(self, fn, reads=(), writes=()):
        return self.add("pool", fn, reads, writes)

    def dma(self, eng, out, in_, reads=(), writes=(), slot=None):
        assert slot is not None
        return self.add(eng, _mk("dma_start", out=out, in_=in_), reads, writes, dma=slot)

    def emit(self, final_keys=(), final_all=False):
        nc = self.nc
        fin = Op("sp", None, None)
        for k in final_keys:
            w = self.lastw.get(k)
            if w is not None:
                fin.deps.append(w)
                w.users += 1
        if final_all:
            for w in self.barrier_list():
                fin.deps.append(w)
                w.users += 1
        sem_ctx = []
        eng_sems = {e: [] for e in ENGS}
        counters = {e: 0 for e in ENGS}
        slot_cnt = {}
        slot_sem = {}
        nsem = [0]

        def new_sem():
            cm = nc.semaphore("sm%d" % nsem[0])
            s = cm.__enter__()
            sem_ctx.append(cm)
            nsem[0] += 1
            return s

        for op in self.order:
            if op.dma is not None:
                if op.dma not in slot_sem or slot_cnt[op.dma] + op.inc > SEM_LIMIT:
                    slot_sem[op.dma] = new_sem()
                    slot_cnt[op.dma] = 0
                slot_cnt[op.dma] += op.inc
                op.sem = slot_sem[op.dma]
                op.val = slot_cnt[op.dma]
            elif op.users > 0:
                e = op.eng
                if not eng_sems[e] or counters[e] + 1 > SEM_LIMIT:
                    eng_sems[e].append(new_sem())
                    counters[e] = 0
                counters[e] += 1
                op.sem = eng_sems[e][-1]
                op.val = counters[e]
        for op in self.order:
            if op.dma is not None and isinstance(op.dma, str) and op.dma.startswith("G:"):
                assert slot_cnt[op.dma] <= SEM_LIMIT
                op.val = slot_cnt[op.dma]
        if os.environ.get("PROG_DEBUG"):
            names = {}
            for op in self.order:
                if op.sem is not None:
                    names.setdefault(id(op.sem), (op.dma if op.dma is not None else op.eng, []))[1].append(op.val)
            for i, (k, (nm, vals)) in enumerate(names.items()):
                print("SEM", i, nm, "n=%d max=%d" % (len(vals), max(vals)))
        waited = {e: {} for e in ENGS}

        def calc_waits(op):
            ws = {}
            for d in op.deps:
                key = id(d.sem)
                if waited[op.eng].get(key, 0) >= d.val:
                    continue
                if key not in ws or ws[key][1] < d.val:
                    ws[key] = (d.sem, d.val)
            for key, (s, v) in ws.items():
                waited[op.eng][key] = v
            return list(ws.values())

        for op in self.order:
            op.waits = calc_waits(op)
        fin.waits = calc_waits(fin)
        self.n_sems = nsem[0]
        engmap = {"pe": "tensor", "act": "scalar", "dve": "vector", "pool": "gpsimd", "sp": "sync"}
        stats = [0]
        self.stats = stats
        with nc.Block() as block:
            for e in ENGS:
                ops = self.ops[e]

                def body(eng, ops=ops, is_last=(e == "sp")):
                    for op in ops:
                        ws = list(op.waits)
                        fused = None
                        if ws and op.dma is None and FUSE_WAITS:
                            fused = ws.pop()
                        for s, v in ws:
                            eng.wait_ge(s, v)
                        ins = op.fn(eng)
                        if fused is not None:
                            ins._wait_ge(fused[0], fused[1])
                        if op.sem is not None:
                            ins.then_inc(op.sem, op.inc)
                        stats[0] += 1 + len(ws)
                    if is_last:
                        for s, v in fin.waits:
                            eng.wait_ge(s, v)

                getattr(block, engmap[e])(body)
        for cm in reversed(sem_ctx):
            cm.__exit__(None, None, None)


class Ctx:
    pass


def _mk(method, *a, **k):
    return lambda e: getattr(e, method)(*a, **k)


def _din(nc, name, shape, dt=F32):
    return nc.dram_tensor(name, list(shape), dt, kind="ExternalInput").ap()


def _dout(nc, name, shape, dt=F32):
    return nc.dram_tensor(name, list(shape), dt, kind="ExternalOutput").ap()


_DTSIZE = {F32: 4, BF16: 2}
ARENA_ELEMS = 106000


def _sb(C, name, shape, dt=F32):
    n = 1
    for v in shape[1:]:
        n *= v
    nbytes = n * _DTSIZE[dt]
    off = (C.arena_off + 63) // 64 * 64
    assert off + nbytes <= ARENA_ELEMS * 2, ("SBUF arena overflow", name, off, nbytes)
    C.arena_off = off + nbytes
    C.arena_peak = max(getattr(C, "arena_peak", 0), C.arena_off)
    ap = C.arena[0:shape[0], off // 2:(off + nbytes) // 2]
    if dt != BF16:
        ap = ap.bitcast(dt)
    if len(shape) == 3:
        ap = ap.rearrange("p (a b) -> p a b", a=shape[1], b=shape[2])
    elif len(shape) != 2:
        raise ValueError(shape)
    return ap


def emit_consts(C):
    P = C.P
    C.ident_b = _sb(C, "ident_b", [128, 128], BF16)
    C.ident_f = _sb(C, "ident_f", [128, 128], F32)
    C.ones_f = _sb(C, "ones_f", [128, 128], F32)
    C.mhalf = _sb(C, "mhalf", [128, 16], F32)
    for t, k in ((C.ident_b, "ident_b"), (C.ident_f, "ident_f")):
        P.pool(_mk("memset", t[:], 0.0), writes=[k])
        P.pool(_mk("affine_select", out=t[:], in_=t[:], pattern=[[-1, 128]],
                                             compare_op=ALU.not_equal, fill=1.0, base=0,
                                             channel_multiplier=1), reads=[k], writes=[k])
    P.pool(_mk("memset", C.ones_f[:], 1.0), writes=["ones_f"])
    P.pool(_mk("memset", C.mhalf[:], -0.5), writes=["mhalf"])


def emit_mod(C, col0, nslab, dst, dkey, wbuf, wkeys):
    P, nc = C.P, C.nc
    cT = _sb(C, "cT_sb", [128, 8], F32)
    cond = _sb(C, "cond_sb", [128, 8], F32)
    lhs = _sb(C, "modlhs", [128, 8, 128], F32)
    bbuf = [_sb(C, "modb%d" % i, [1, 512], F32) for i in range(2)]
    P.dma("sp", cT[:], C.d["cT"], writes=["cT"], slot="cT")
    P.act(_mk("activation", out=cond[:], in_=cT[:], func=AF.Silu), reads=["cT"], writes=["cond"])
    P.dve(_mk("tensor_copy", out=lhs[:], in_=cond[:].unsqueeze(2).to_broadcast([128, 8, 128])),
          reads=["cond"], writes=["modlhs"])
    for s in range(nslab):
        b = s % 2
        c0 = col0 + s * 512
        P.dma("sp", wbuf[b], C.d["w_ada"][:, c0:c0 + 512].rearrange("(c p) n -> p c n", p=128),
              writes=list(wkeys[b]), slot=("modw", b))
        P.dma("sp", bbuf[b][:], C.d["b_ada"][0:1, c0:c0 + 512], writes=[("modb", b)], slot=("modb", b))
        pk = ("ps", s % 2)
        ps = C.ps[s % 2]
        for c in range(8):
            P.pe(_mk("matmul", ps[:], lhsT=lhs[:, c, :], rhs=wbuf[b][:, c, :],
                                                   start=(c == 0), stop=False),
                 reads=["modlhs"] + list(wkeys[b]), writes=[pk])
        P.pe(_mk("matmul", ps[:], lhsT=C.ones_f[0:1, :], rhs=bbuf[b][0:1, :],
                                            start=False, stop=True),
             reads=["ones_f", ("modb", b)], writes=[pk])
        P.act(_mk("copy", out=dst[:, s * 512:(s + 1) * 512], in_=ps[:]),
              reads=[pk], writes=[dkey])


def emit_ln_tile(C, xr, xrkey, g, b, gbkey, out, outkey, eps=1e-5):
    P = C.P
    st, mv, rs = C.ln_st, C.ln_mv, C.ln_rs
    for i in range(2):
        P.dve(_mk("bn_stats", out=st[:, i, :], in_=xr[:, i * 512:(i + 1) * 512]),
              reads=[xrkey], writes=[("ln_st", i)])
    P.dve(_mk("bn_aggr", out=mv[:], in_=st[:]), reads=[("ln_st", 0), ("ln_st", 1)], writes=["ln_mv"])
    P.dve(_mk("tensor_scalar_add", out=rs[:], in0=mv[:, 1:2], scalar1=eps), reads=["ln_mv"], writes=["ln_rs"])
    P.pool(_mk("tensor_tensor", out=rs[:], in0=rs[:], in1=C.mhalf[:, 0:1], op=ALU.pow),
           reads=["ln_rs", "mhalf"], writes=["ln_rs"])
    P.dve(_mk("tensor_scalar", out=xr, in0=xr, scalar1=mv[:, 0:1], scalar2=rs[:, 0:1],
                                    op0=ALU.subtract, op1=ALU.mult),
          reads=[xrkey, "ln_mv", "ln_rs"], writes=[xrkey])
    P.pool(_mk("tensor_tensor", out=xr, in0=xr, in1=g, op=ALU.mult), reads=[xrkey] + list(gbkey), writes=[xrkey])
    P.pool(_mk("tensor_tensor", out=out, in0=xr, in1=b, op=ALU.add), reads=[xrkey] + list(gbkey), writes=[outkey])


def emit_normrope(C, ps_ap, pskey, hi, lo, gsb, gkey, cs, sn, cskey, out_view, outkey):
    P = C.P
    nh = hi * lo
    W = nh * 64
    sq, sqk = C.F[0], ("F", 0)
    qn, qnk = C.F[1], ("F", 1)
    ss = C.nr_ss
    tA, tB, tC, tD = C.nr_t
    P.act(_mk("activation", out=sq[:, 0:W], in_=ps_ap, func=AF.Square), reads=[pskey], writes=[sqk])
    P.dve(_mk("tensor_reduce", out=ss[:, 0:nh], in_=sq[:, 0:W].rearrange("p (h d) -> p h d", d=64),
                                    axis=AX.X, op=ALU.add), reads=[sqk], writes=["nr_ss"])
    P.dve(_mk("tensor_scalar", out=ss[:, 0:nh], in0=ss[:, 0:nh], scalar1=1.0 / 64, scalar2=1e-6,
                                    op0=ALU.mult, op1=ALU.add), reads=["nr_ss"], writes=["nr_ss"])
    P.pool(_mk("tensor_tensor", out=ss[:, 0:nh], in0=ss[:, 0:nh], in1=C.mhalf[:, 0:nh], op=ALU.pow),
           reads=["nr_ss", "mhalf"], writes=["nr_ss"])
    P.dve(_mk("tensor_tensor", out=qn[:, 0:W].rearrange("p (h d) -> p h d", d=64),
                                    in0=ps_ap.rearrange("p (h d) -> p h d", d=64),
                                    in1=ss[:, 0:nh].unsqueeze(2).to_broadcast([128, nh, 64]), op=ALU.mult),
          reads=[pskey, "nr_ss"], writes=[qnk])
    P.pool(_mk("tensor_tensor", out=qn[:, 0:W], in0=qn[:, 0:W], in1=gsb, op=ALU.mult),
           reads=[qnk, gkey], writes=[qnk])
    qv = qn[:, 0:W].rearrange("p (a b i two) -> p a b i two", a=hi, b=lo, two=2)
    x0 = qv[:, :, :, :, 0]
    x1 = qv[:, :, :, :, 1]
    cb = cs.unsqueeze(1).unsqueeze(1).to_broadcast([128, hi, lo, 32])
    sb_ = sn.unsqueeze(1).unsqueeze(1).to_broadcast([128, hi, lo, 32])

    def v4(t):
        return t[:, 0:nh * 32].rearrange("p (a b i) -> p a b i", a=hi, b=lo)

    P.dve(_mk("tensor_tensor", out=v4(tA), in0=x0, in1=cb, op=ALU.mult), reads=[qnk] + list(cskey), writes=["nr_tA"])
    P.dve(_mk("tensor_tensor", out=v4(tB), in0=x1, in1=sb_, op=ALU.mult), reads=[qnk] + list(cskey), writes=["nr_tB"])
    P.dve(_mk("tensor_tensor", out=out_view[:, :, :, :, 0], in0=v4(tA), in1=v4(tB), op=ALU.subtract),
          reads=["nr_tA", "nr_tB"], writes=[outkey + "_e"])
    P.pool(_mk("tensor_tensor", out=v4(tC), in0=x0, in1=sb_, op=ALU.mult), reads=[qnk] + list(cskey), writes=["nr_tC"])
    P.pool(_mk("tensor_tensor", out=v4(tD), in0=x1, in1=cb, op=ALU.mult), reads=[qnk] + list(cskey), writes=["nr_tD"])
    P.pool(_mk("tensor_tensor", out=out_view[:, :, :, :, 1], in0=v4(tC), in1=v4(tD), op=ALU.add),
           reads=["nr_tC", "nr_tD"], writes=[outkey + "_o"])


def alloc_common(C):
    C.hf = _sb(C, "hf", [128, 1024], F32)
    C.hb = _sb(C, "hb", [128, 1024], BF16)
    C.ln_st = _sb(C, "ln_st", [128, 2, 6], F32)
    C.ln_mv = _sb(C, "ln_mv", [128, 2], F32)
    C.ln_rs = _sb(C, "ln_rs", [128, 1], F32)
    C.F = [_sb(C, "F%d" % i, [128, 512], F32) for i in range(4)]


def alloc_normrope(C):
    C.nr_ss = _sb(C, "nr_ss", [128, 8], F32)
    C.nr_t = [_sb(C, "nr_t%d" % i, [128, 256], F32) for i in range(4)]


def emit_hT_block(C, blk, xsrc, sc1p, sh1, hT):
    P = C.P
    for t in range(4):
        T = blk * 4 + t
        xt, xkey = xsrc(T)
        P.dve(_mk("tensor_tensor", out=C.hf[:], in0=xt, in1=sc1p[:], op=ALU.mult),
              reads=[xkey, "mod1"], writes=["hf"])
        P.pool(_mk("tensor_tensor", out=C.hb[:], in0=C.hf[:], in1=sh1[:], op=ALU.add),
               reads=["hf", "mod1"], writes=["hb"])
        pb = t % 2
        pk = ("ps", pb)
        psv = C.ps[pb][:].bitcast(BF16)
        for c in range(8):
            P.pe(_mk("transpose", out=psv[:, c * 128:(c + 1) * 128],
                                                     in_=C.hb[:, c * 128:(c + 1) * 128], identity=C.ident_b[:]),
                 reads=["hb", "ident_b"], writes=[pk])
        P.act(_mk("copy", out=hT[:, :, t * 128:(t + 1) * 128],
                                             in_=psv.rearrange("p (c k) -> p c k", c=8)),
              reads=[pk], writes=[("hT", t)])


def emit_kv(C, d):
    P = C.P
    nc = C.nc
    C.d = d
    mod = _sb(C, "mod1", [128, 2048], F32)
    mw = [_sb(C, "modw%d" % i, [128, 8, 512], F32) for i in range(2)]
    sh1 = mod[:, 0:1024]
    sc1p = mod[:, 1024:2048]
    wkv = _sb(C, "wkv", [128, 8, 256], BF16)
    P.dma("pool", wkv[:], d["w_in"][:, 512:768].rearrange("(c p) n -> p c n", p=128), writes=["wkv"], slot=C.gp)
    kg = _sb(C, "kg", [128, 128], F32)
    P.dma("sp", kg[:], d["kg"], writes=["kg"], slot=C.g)
    cs = _sb(C, "cos", [128, NT, 32], F32)
    sn = _sb(C, "sin", [128, NT, 32], F32)
    P.dma("sp", cs[:], d["cos"], writes=["cs"], slot=C.g)
    P.dma("sp", sn[:], d["sin"], writes=["sn"], slot=C.g)
    emit_mod(C, 0, 4, mod, "mod1", [mw[0][:], mw[1][:]], [[("modw", 0)], [("modw", 1)]])
    P.dve(_mk("tensor_scalar_add", out=sc1p, in0=sc1p, scalar1=1.0), reads=["mod1"], writes=["mod1"])
    xt = [_sb(C, "xt%d" % i, [128, 1024], F32) for i in range(2)]
    hT = _sb(C, "hT", [128, 8, 512], BF16)
    kr = _sb(C, "kr", [128, 128], BF16)
    kT = _sb(C, "kT", [128, TOK], BF16)
    va = _sb(C, "va", [128, NT, 130], BF16)
    P.pool(_mk("memset", va[:], 1.0), writes=["va"])

    def xsrc(T):
        b = T % 2
        P.dma("sp", xt[b][:], d["x_in"][T * 128:(T + 1) * 128, :], writes=[("xt", b)], slot=("xt", b))
        return xt[b][:], ("xt", b)

    for blk in range(NBLK):
        emit_hT_block(C, blk, xsrc, sc1p, sh1, hT)
        for t in range(4):
            T = blk * 4 + t
            pb = 2 + t % 2
            pk = ("ps", pb)
            ps = C.ps[pb]
            for c in range(8):
                P.pe(_mk("matmul", ps[:, 0:256], lhsT=hT[:, c, t * 128:(t + 1) * 128],
                                                       rhs=wkv[:, c, :], start=(c == 0), stop=(c == 7)),
                     reads=[("hT", t), "wkv"], writes=[pk])
            P.act(_mk("copy", out=va[:, T, :].rearrange("p (h d) -> p h d", h=2)[:, :, 0:64],
                                               in_=ps[:, 128:256].rearrange("p (h d) -> p h d", h=2)),
                  reads=[pk, "va"], writes=["va"])
            emit_normrope(C, ps[:, 0:128], pk, 2, 1, kg[:], "kg", cs[:, T, :], sn[:, T, :], ["cs", "sn"],
                          kr[:].rearrange("p (a b i two) -> p a b i two", a=2, b=1, two=2), "kr")
            pb2 = 4 + t % 2
            pk2 = ("ps", pb2)
            psv = C.ps[pb2][:].bitcast(BF16)
            P.pe(_mk("transpose", out=psv[:, 0:128], in_=kr[:], identity=C.ident_b[:]),
                 reads=["kr_e", "kr_o", "ident_b"], writes=[pk2])
            P.act(_mk("copy", out=kT[:, T * 128:(T + 1) * 128], in_=psv[:, 0:128]),
                  reads=[pk2], writes=["kT"])
    P.dma("sp", d["kT_out"], kT[:], reads=["kT"], writes=["kT_out"], slot="kT_out")
    P.dma("sp", d["v_out"].rearrange("(t p) f -> p t f", p=128), va[:], reads=["va"], writes=["v_out"], slot="v_out")


def emit_mix(C, d):
    P = C.P
    nc = C.nc
    C.d = d
    NR = 6
    ring = _sb(C, "ring", [128, NR * 4096], BF16)
    mod = _sb(C, "mod1", [128, 3072], F32)
    mw = [ring[:, b * 8192:(b + 1) * 8192].bitcast(F32).rearrange("p (c n) -> p c n", c=8) for b in range(2)]
    sh1 = mod[:, 0:1024]
    sc1p = mod[:, 1024:2048]
    gate1 = mod[:, 2048:3072]

    def small(name, shape, dt=F32, src=None, eng="sp"):
        t = _sb(C, name, shape, dt)
        P.dma(eng, t[:], d[name] if src is None else src, writes=[name], slot=C.g if eng == "sp" else C.gp)
        return t

    qg = small("qg", [128, 512])
    cs = small("cos", [128, NT, 32])
    sn = small("sin", [128, NT, 32])
    lng = small("lng", [128, 512])
    lnb = small("lnb", [128, 512])
    bsT = small("bsT", [128, 4, 128])
    ln1g = small("ln1g", [128, D])
    ln1b = small("ln1b", [128, D])
    kT = _sb(C, "kT_full", [128, SEQ], BF16)
    for r in range(2):
        P.dma("sp", kT[:, r * TOK:(r + 1) * TOK], d["kT_all"][r * 128:(r + 1) * 128, :], writes=[("kT_full", r)], slot=C.g)
    vsb = _sb(C, "v_full", [128, 32, 130], BF16)
    for i in range(4):
        P.dma("sp", vsb[:, i * 8:(i + 1) * 8, :],
              d["v_full"][i * 1024:(i + 1) * 1024, :].rearrange("(t p) f -> p t f", p=128),
              writes=[("v_full", i)], slot=C.g)
    ones_b = _sb(C, "ones_b", [128, 64], BF16)
    P.pool(_mk("memset", ones_b[:], 1.0), writes=["ones_b"])
    emit_mod(C, 0, 6, mod, "mod1", mw, [[("slab", 0), ("slab", 1)], [("slab", 2), ("slab", 3)]])
    P.dve(_mk("tensor_scalar_add", out=sc1p, in0=sc1p, scalar1=1.0), reads=["mod1"], writes=["mod1"])
    wsp = small("w_sp", [128, 8, 128], BF16, src=d["w_sp"].rearrange("g i j -> i g j"), eng="pool")
    wsT = _sb(C, "wsT", [128, 8, 128], BF16)
    psv = C.ps[2][:].bitcast(BF16)
    for g in range(8):
        P.pe(_mk("transpose", out=psv[:, g * 128:(g + 1) * 128], in_=wsp[:, g, :], identity=C.ident_b[:]),
             reads=["w_sp", "ident_b"], writes=[("ps", 2)])
    P.act(_mk("copy", out=wsT[:].rearrange("p g i -> p (g i)"), in_=psv), reads=[("ps", 2)], writes=["wsT"])

    q_pad = _sb(C, "q_pad", [128, 8, 512], BF16)
    P.pool(_mk("memset", q_pad[:], 0.0), writes=["q_pad"])
    NXT = 4
    xt = [_sb(C, "xt%d" % i, [128, 1024], F32) for i in range(NXT)]
    hT = _sb(C, "hT", [128, 8, 512], BF16)
    q_r = _sb(C, "q_r", [128, 512], BF16)
    uT = _sb(C, "uT", [128, 4, 512], BF16)
    vnb = _sb(C, "vnb", [128, 512], BF16)
    o_bT = _sb(C, "o_bT", [128, 4, 512], BF16)
    pT = [_sb(C, "pT%d" % i, [128, 512], BF16) for i in range(3)]
    o_aT = _sb(C, "o_aT", [128, 8, 512], BF16)
    sg = [[_sb(C, "sg%d%d" % (i, j), [128, 512], BF16) for j in range(2)] for i in range(2)]
    yT = _sb(C, "yT", [128, 8, 512], BF16)
    xo = _sb(C, "xo", [128, 1024], F32)
    sg_st = _sb(C, "sg_st", [128, 6], F32)
    sg_mv = _sb(C, "sg_mv", [128, 2], F32)
    sg_rs = _sb(C, "sg_rs", [128, 1], F32)

    def cp(ap):
        return ap.rearrange("(c p) n -> p c n", p=128)

    w_in = d["w_in"]
    specs = [
        ("q", cp(w_in[:, 0:512]), 128, 8),
        ("zv", cp(w_in[:, 1280:1792]), 128, 8),
        ("zu", cp(w_in[:, 768:1280]), 128, 8),
        ("wa0", d["wa"][:, 0:512].rearrange("(h k) n -> k h n", k=64), 64, 8),
        ("wb", cp(d["wb"]), 128, 4),
        ("ga0", cp(w_in[:, 1792:2304]), 128, 8),
        ("gb0", cp(w_in[:, 2816:3328]), 128, 8),
        ("wa1", d["wa"][:, 512:1024].rearrange("(h k) n -> k h n", k=64), 64, 8),
        ("ga1", cp(w_in[:, 2304:2816]), 128, 8),
        ("gb1", cp(w_in[:, 3328:3840]), 128, 8),
        ("wo0", cp(d["wo"][:, 0:512]), 128, 8),
        ("wo1", cp(d["wo"][:, 512:1024]), 128, 8),
    ]
    NS = len(specs)
    total = NBLK * NS
    state = {"next": 0}

    def slab_view(gi):
        name, src, npart, c = specs[gi % NS]
        b = gi % NR
        return ring[0:npart, b * 4096:(b + 1) * 4096].rearrange("p (c n) -> p c n", c=c), ("slab", b)

    def ensure(gi):
        while state["next"] <= min(gi, total - 1):
            g = state["next"]
            name, src, npart, c = specs[g % NS]
            v, key = slab_view(g)
            P.dma("pool", v, src, writes=[key], slot=key)
            state["next"] += 1

    def slab(blk, j):
        gi = blk * NS + j
        assert gi < state["next"], (gi, state["next"])
        return slab_view(gi)

    def stage(blk, lo):
        ensure(blk * NS + lo + NR - 1)

    def xsrc(T):
        b = T % NXT
        P.dma("sp", xt[b][:], d["x_in"][T * 128:(T + 1) * 128, :], writes=[("xt", b)], slot=("xt", b))
        return xt[b][:], ("xt", b)

    hTk = [("hT", t) for t in range(4)]
    STOP = int(os.environ.get("MIX_STOP", "99"))
    for blk in range(NBLK if STOP > 0 else 0):
        emit_hT_block(C, blk, xsrc, sc1p, sh1, hT)
        if STOP <= 1:
            break
        stage(blk, 0)
        wq, wqk = slab(blk, 0)
        for t in range(4):
            T = blk * 4 + t
            pb = 2 + t % 2
            pk = ("ps", pb)
            ps = C.ps[pb]
            for c in range(8):
                P.pe(_mk("matmul", ps[:], lhsT=hT[:, c, t * 128:(t + 1) * 128], rhs=wq[:, c, :],
                                                       start=(c == 0), stop=(c == 7)),
                     reads=[("hT", t), wqk], writes=[pk])
            emit_normrope(C, ps[:], pk, 2, 4, qg[:], "qg", cs[:, T, :], sn[:, T, :], ["cos", "sin"],
                          q_r[:].rearrange("p (b a i two) -> p a b i two", a=2, b=4, two=2), "q_r")
            pb2 = 4 + t % 2
            pk2 = ("ps", pb2)
            pv = C.ps[pb2][:].bitcast(BF16)
            for j in range(4):
                P.pe(_mk("transpose", out=pv[:, j * 128:(j + 1) * 128], in_=q_r[:, j * 128:(j + 1) * 128],
                                                       identity=C.ident_b[:]),
                     reads=["q_r_e", "q_r_o", "ident_b"], writes=[pk2])
            pv3 = pv[:, 0:512].rearrange("p (j k) -> p j k", j=4)
            P.act(_mk("copy", out=q_pad[0:64, 0:4, t * 128:(t + 1) * 128], in_=pv3[0:64]),
                  reads=[pk2], writes=[("q_pad", 0, t)])
            P.dve(_mk("tensor_copy", out=q_pad[64:128, 4:8, t * 128:(t + 1) * 128], in_=pv3[64:128]),
                  reads=[pk2], writes=[("q_pad", 1, t)])
        if STOP <= 2:
            break
        qpk = ["q_pad"] + [("q_pad", i, t) for i in range(2) for t in range(4)]
        stage(blk, 1)
        wzv, wzvk = slab(blk, 1)
        wzu, wzuk = slab(blk, 2)
        for cc in range(4):
            pb = 6 + cc % 2
            pk = ("ps", pb)
            ps = C.ps[pb]
            for c in range(8):
                P.pe(_mk("matmul", ps[:], lhsT=wzu[:, c, cc * 128:(cc + 1) * 128], rhs=hT[:, c, :],
                                                         start=(c == 0), stop=(c == 7)),
                     reads=hTk + [wzuk], writes=[pk])
            P.act(_mk("activation", out=uT[:, cc, :], in_=ps[:], func=AF.Gelu),
                  reads=[pk], writes=[("uT", cc)])
        uTk = [("uT", cc) for cc in range(4)]
        vg, vgk = C.F[2], ("F", 2)
        stmp, stmpk = C.F[3], ("F", 3)
        for t in range(4):
            pb = 2 + t % 2
            pk = ("ps", pb)
            ps = C.ps[pb]
            for c in range(8):
                P.pe(_mk("matmul", ps[:], lhsT=hT[:, c, t * 128:(t + 1) * 128], rhs=wzv[:, c, :],
                                                       start=(c == 0), stop=(c == 7)),
                     reads=[("hT", t), wzvk], writes=[pk])
            P.act(_mk("activation", out=vg[:], in_=ps[:], func=AF.Gelu), reads=[pk], writes=[vgk])
            P.dve(_mk("bn_stats", out=sg_st[:], in_=vg[:]), reads=[vgk], writes=["sg_st"])
            P.dve(_mk("bn_aggr", out=sg_mv[:], in_=sg_st[:]), reads=["sg_st"], writes=["sg_mv"])
            P.dve(_mk("tensor_scalar_add", out=sg_rs[:], in0=sg_mv[:, 1:2], scalar1=1e-5), reads=["sg_mv"], writes=["sg_rs"])
            P.pool(_mk("tensor_tensor", out=sg_rs[:], in0=sg_rs[:], in1=C.mhalf[:, 0:1], op=ALU.pow),
                   reads=["sg_rs", "mhalf"], writes=["sg_rs"])
            P.dve(_mk("tensor_scalar", out=vg[:], in0=vg[:], scalar1=sg_mv[:, 0:1], scalar2=sg_rs[:, 0:1],
                                            op0=ALU.subtract, op1=ALU.mult), reads=[vgk, "sg_mv", "sg_rs"], writes=[vgk])
            P.pool(_mk("tensor_tensor", out=vg[:], in0=vg[:], in1=lng[:], op=ALU.mult), reads=[vgk, "lng"], writes=[vgk])
            P.pool(_mk("tensor_tensor", out=vnb[:], in0=vg[:], in1=lnb[:], op=ALU.add), reads=[vgk, "lnb"], writes=["vnb"])
            pb2 = 4 + t % 2
            pk2 = ("ps", pb2)
            ps2 = C.ps[pb2]
            for g in range(8):
                P.pe(_mk("matmul", ps2[(g % 2) * 64:(g % 2) * 64 + 64, (g // 2) * 128:(g // 2) * 128 + 128],
                                                      lhsT=vnb[:, g * 64:(g + 1) * 64], rhs=wsT[:, g, :],
                                                      start=True, stop=True),
                     reads=["vnb", "wsT"], writes=[pk2])
            P.dve(_mk("tensor_tensor", out=stmp[:].rearrange("p (c i) -> p c i", c=4),
                                                     in0=ps2[:].rearrange("p (c i) -> p c i", c=4), in1=bsT[:], op=ALU.add),
                  reads=[pk2, "bsT"], writes=[stmpk])
            P.dve(_mk("tensor_tensor", out=o_bT[:, :, t * 128:(t + 1) * 128],
                                                 in0=stmp[:].rearrange("p (c i) -> p c i", c=4),
                                                 in1=uT[:, :, t * 128:(t + 1) * 128], op=ALU.mult),
                  reads=[stmpk] + uTk, writes=[("o_bT", t)])
        obk = [("o_bT", t) for t in range(4)]
        if STOP <= 3:
            break
        rden, rdenk = C.F[3], ("F", 3)
        stage(blk, 3)
        pending = [None]

        def tail(h, accb):
            acc = C.ps[accb]
            ak = ("ps", accb)
            den = C.ps[accb + 2]
            dk = ("ps", accb + 2)
            P.dve(_mk("reciprocal", out=rden[0:64, :], in_=den[0:64, :]), reads=[dk], writes=[rdenk])
            P.dve(_mk("tensor_tensor", out=o_aT[0:64, h, :], in0=acc[0:64, :], in1=rden[0:64, :], op=ALU.mult),
                  reads=[ak, rdenk], writes=[("o_aT", h)])

        for h in range(8):
            kvh = h // 4
            accb = 3 + h % 2
            acc = C.ps[accb]
            ak = ("ps", accb)

            def s_mm(kt, h=h):
                sbk = kt % 3
                P.pe(_mk("matmul", C.ps[sbk][:], lhsT=kT[:, kt * 128:(kt + 1) * 128], rhs=q_pad[:, h, :],
                                        start=True, stop=True),
                     reads=[("kT_full", 0), ("kT_full", 1)] + qpk, writes=[("ps", sbk)])
                P.act(_mk("activation", out=pT[sbk][:], in_=C.ps[sbk][:], func=AF.Exp, scale=0.125),
                      reads=[("ps", sbk)], writes=[("pT", sbk)])

            def pv_mm(kt, kvh=kvh, acc=acc, ak=ak, accb=accb):
                sbk = kt % 3
                P.pe(_mk("matmul", acc[0:64, :], lhsT=vsb[:, kt, kvh * 65:kvh * 65 + 64], rhs=pT[sbk][:],
                                        start=(kt == 0), stop=(kt == 31)),
                     reads=[("pT", sbk)] + [("v_full", i) for i in range(4)], writes=[ak])
                P.pe(_mk("matmul", C.ps[accb + 2][0:64, :], lhsT=ones_b[:, :], rhs=pT[sbk][:],
                                        start=(kt == 0), stop=(kt == 31)),
                     reads=[("pT", sbk), "ones_b"], writes=[("ps", accb + 2)])

            s_mm(0)
            s_mm(1)
            if pending[0] is not None:
                tail(*pending[0])
            for kt in range(32):
                if kt + 2 < 32:
                    s_mm(kt + 2)
                pv_mm(kt)
            pending[0] = (h, accb)
        tail(*pending[0])
        oak = [("o_aT", h) for h in range(8)]
        if STOP <= 4:
            break
        t1, t1k = C.F[0], ("F", 0)
        t2, t2k = C.F[1], ("F", 1)
        for m in range(8):
            half = m // 4
            if m == 4:
                stage(blk, 4)
            mm = m % 4
            wa_s, wak = slab(blk, 3 if half == 0 else 7)
            wb_s, wbk = slab(blk, 4)
            ga_s, gak = slab(blk, 5 if half == 0 else 8)
            gb_s, gbk = slab(blk, 6 if half == 0 else 9)
            par = m % 2
            b_ya, b_ga, b_yb, b_gb = [4 * par + i for i in range(4)]
            for h in range(8):
                P.pe(_mk("matmul", C.ps[b_ya][:], lhsT=wa_s[0:64, h, mm * 128:(mm + 1) * 128], rhs=o_aT[0:64, h, :],
                                             start=(h == 0), stop=(h == 7)),
                     reads=oak + [wak], writes=[("ps", b_ya)])
            for c in range(8):
                P.pe(_mk("matmul", C.ps[b_ga][:], lhsT=ga_s[:, c, mm * 128:(mm + 1) * 128], rhs=hT[:, c, :],
                                             start=(c == 0), stop=(c == 7)),
                     reads=hTk + [gak], writes=[("ps", b_ga)])
            for cc in range(4):
                P.pe(_mk("matmul", C.ps[b_yb][:], lhsT=wb_s[:, cc, m * 128:(m + 1) * 128], rhs=o_bT[:, cc, :],
                                               start=(cc == 0), stop=(cc == 3)),
                     reads=obk + [wbk], writes=[("ps", b_yb)])
            for c in range(8):
                P.pe(_mk("matmul", C.ps[b_gb][:], lhsT=gb_s[:, c, mm * 128:(mm + 1) * 128], rhs=hT[:, c, :],
                                             start=(c == 0), stop=(c == 7)),
                     reads=hTk + [gbk], writes=[("ps", b_gb)])
            P.act(_mk("activation", out=sg[par][0][:], in_=C.ps[b_ga][:], func=AF.Sigmoid),
                  reads=[("ps", b_ga)], writes=[("sg", par, 0)])
            P.act(_mk("activation", out=sg[par][1][:], in_=C.ps[b_gb][:], func=AF.Sigmoid),
                  reads=[("ps", b_gb)], writes=[("sg", par, 1)])
            P.dve(_mk("tensor_tensor", out=t1[:], in0=C.ps[b_ya][:], in1=sg[par][0][:], op=ALU.mult),
                  reads=[("ps", b_ya), ("sg", par, 0)], writes=[t1k])
            P.dve(_mk("tensor_tensor", out=t2[:], in0=C.ps[b_yb][:], in1=sg[par][1][:], op=ALU.mult),
                  reads=[("ps", b_yb), ("sg", par, 1)], writes=[t2k])
            P.pool(_mk("tensor_tensor", out=yT[:, m, :], in0=t1[:], in1=t2[:], op=ALU.add),
                   reads=[t1k, t2k], writes=[("yT", m)])
        yTk = [("yT", m) for m in range(8)]
        xr, xrk = C.hf, "hf"
        stage(blk, 10)
        for t in range(4):
            T = blk * 4 + t
            for half in range(2):
                wo_s, wok = slab(blk, 10 + half)
                pb = (t * 2 + half) % 4
                for m in range(8):
                    P.pe(_mk("matmul", C.ps[pb][:], lhsT=yT[:, m, t * 128:(t + 1) * 128], rhs=wo_s[:, m, :],
                                                     start=(m == 0), stop=(m == 7)),
                         reads=yTk + [wok], writes=[("ps", pb)])
                P.dve(_mk("tensor_tensor", out=xr[:, half * 512:(half + 1) * 512], in0=C.ps[pb][:],
                                                           in1=gate1[:, half * 512:(half + 1) * 512], op=ALU.mult),
                      reads=[("ps", pb), "mod1"], writes=[xrk])
            xb = T % NXT
            P.dve(_mk("scalar_tensor_tensor", out=xr[:], in0=xt[xb][:], scalar=float(ALPHA), in1=xr[:],
                                                          op0=ALU.mult, op1=ALU.add),
                  reads=[("xt", xb), xrk], writes=[xrk])
            emit_ln_tile(C, xr[:], xrk, ln1g[:], ln1b[:], ["ln1g", "ln1b"], xo[:], "xo")
            P.dma("sp", d["x_out"][T * 128:(T + 1) * 128, :], xo[:], reads=["xo"], writes=["x_out"], slot="x_out")
        if STOP <= 6:
            break


def emit_moe(C, d):
    P = C.P
    nc = C.nc
    C.d = d
    NE = NEXP
    X = _sb(C, "X", [128, NT, D], F32)
    h2T = _sb(C, "h2T", [128, 8, TOK], BF16)
    mod = _sb(C, "mod2", [128, 3072], F32)
    sh2 = mod[:, 0:1024]
    sc2p = mod[:, 1024:2048]
    gate2 = mod[:, 2048:3072]
    G = _sb(C, "G", [128, NT, NEXP], F32)
    ln2g = _sb(C, "ln2g", [128, D], F32)
    ln2b = _sb(C, "ln2b", [128, D], F32)
    rb = _sb(C, "rb", [128, NEXP], F32)
    rw = _sb(C, "rw", [128, 8, NEXP], F32)
    h32T = _sb(C, "h32T", [128, 8, 128], F32)
    wgb = [_sb(C, "wgb%d" % i, [128, 8, 256], BF16) for i in range(2)]
    wub = [_sb(C, "wub%d" % i, [128, 8, 256], BF16) for i in range(2)]
    wdst = _sb(C, "wdst", [128, 2, D], F32)
    wdb = [_sb(C, "wdb%d" % i, [128, 2, D], BF16) for i in range(2)]
    sgt = [_sb(C, "sgt%d" % i, [128, 512], BF16) for i in range(2)]
    hbuf = [_sb(C, "hbuf%d" % i, [128, 2, 512], BF16) for i in range(2)]
    xo = [_sb(C, "xo%d" % i, [128, D], F32) for i in range(2)]
    r_sc = _sb(C, "r_sc", [128, NEXP], F32)
    r_sel = _sb(C, "r_sel", [128, NEXP], F32)
    r_eq = _sb(C, "r_eq", [128, NEXP], F32)
    r_sel2 = _sb(C, "r_sel2", [128, NEXP], F32)
    r_m1 = _sb(C, "r_m1", [128, 8], F32)
    r_m2 = _sb(C, "r_m2", [128, 8], F32)
    r_g8 = _sb(C, "r_g8", [128, 8], F32)
    r_gm = _sb(C, "r_gm", [128, 8], F32)
    r_e8 = _sb(C, "r_e8", [128, 8], F32)
    r_ss = _sb(C, "r_ss", [128, 1], F32)

    P.dma("sp", ln2g[:], d["ln2g"], writes=["ln2g"], slot=C.g)
    P.dma("sp", ln2b[:], d["ln2b"], writes=["ln2b"], slot=C.g)
    P.dma("sp", rb[:], d["rb"], writes=["rb"], slot=C.g)
    P.dma("sp", rw[:], d["rw"].rearrange("(p c) e -> p c e", c=8), writes=["rw"], slot=C.g)
    xin = d["x_in"].rearrange("(t p) f -> p t f", p=128)
    for i in range(4):
        P.dma("sp", X[:, i * 4:(i + 1) * 4, :], xin[:, i * 4:(i + 1) * 4, :],
              writes=[("X", T) for T in range(i * 4, i * 4 + 4)], slot=C.g)
    mw = [h2T[:, 0:8, :].rearrange("p c t -> p (c t)")[:, b * 8192:(b + 1) * 8192].bitcast(F32)
          .rearrange("p (c n) -> p c n", c=8) for b in range(2)]
    h2k = [("h2T", T) for T in range(NT)]
    emit_mod(C, 3072, 6, mod, "mod2", mw, [h2k, h2k])
    P.dve(_mk("tensor_scalar_add", out=sc2p, in0=sc2p, scalar1=1.0), reads=["mod2"], writes=["mod2"])

    def load_expert(e):
        b = e % 2
        if e < NEXP:
            sg_, su_, sd_ = d["wg"][e], d["wu"][e], d["wd"][e]
        else:
            sg_, su_, sd_ = d["wsg"], d["wsu"], d["wsd"]
        P.dma("pool", wgb[b][:], sg_.rearrange("(p c) f -> p c f", c=8), writes=[("wgb", b)], slot=("wgb", b))
        P.dma("pool", wub[b][:], su_.rearrange("(p c) f -> p c f", c=8), writes=[("wub", b)], slot=("wub", b))
        P.dma("sp", wdst[:], sd_.rearrange("(c p) n -> p c n", p=128), writes=["wdst"], slot="wdst")
        P.pool(_mk("tensor_tensor", out=wdb[b][:], in0=wdst[:], in1=gate2.unsqueeze(1).to_broadcast([128, 2, D]),
                   op=ALU.mult), reads=["wdst", "mod2"], writes=[("wdb", b)])

    hf = C.hf
    for T in range(NT):
        P.dve(_mk("tensor_tensor", out=hf[:], in0=X[:, T, :], in1=sc2p, op=ALU.mult), reads=[("X", T), "mod2"], writes=["hf"])
        P.pool(_mk("tensor_tensor", out=hf[:], in0=hf[:], in1=sh2, op=ALU.add), reads=["hf", "mod2"], writes=["hf"])
        P.act(_mk("mul", out=X[:, T, :], in_=X[:, T, :], mul=float(ALPHA)), reads=[("X", T)], writes=[("X", T)])
        hv = hf[:].rearrange("t (p c) -> t c p", c=8)
        pa, pb = 2 * (T % 2), 2 * (T % 2) + 1
        for c in range(8):
            bank = pa if c < 4 else pb
            P.pe(_mk("transpose", out=C.ps[bank][:, (c % 4) * 128:(c % 4 + 1) * 128], in_=hv[:, c, :], identity=C.ident_f[:]),
                 reads=["hf", "ident_f"], writes=[("ps", bank)])
        P.act(_mk("copy", out=h32T[:, 0:4, :], in_=C.ps[pa][:].rearrange("p (c k) -> p c k", c=4)),
              reads=[("ps", pa)], writes=["h32a"])
        P.act(_mk("copy", out=h32T[:, 4:8, :], in_=C.ps[pb][:].rearrange("p (c k) -> p c k", c=4)),
              reads=[("ps", pb)], writes=["h32b"])
        P.pool(_mk("tensor_copy", out=h2T[:, :, T * 128:(T + 1) * 128], in_=h32T[:]), reads=["h32a", "h32b"], writes=[("h2T", T)])
        rbk = 4 + T % 2
        for c in range(8):
            P.pe(_mk("matmul", C.ps[rbk][:, 0:NEXP], lhsT=h32T[:, c, :], rhs=rw[:, c, :], start=(c == 0), stop=(c == 7)),
                 reads=["h32a", "h32b", "rw"], writes=[("ps", rbk)])
        P.act(_mk("activation", out=r_sc[:], in_=C.ps[rbk][:, 0:NEXP], func=AF.Sigmoid), reads=[("ps", rbk)], writes=["r_sc"])
        P.dve(_mk("tensor_tensor", out=r_sel[:], in0=r_sc[:], in1=rb[:], op=ALU.add), reads=["r_sc", "rb"], writes=["r_sel"])
        sel3 = r_sel[:].rearrange("p (g k) -> p g k", k=8)
        P.dve(_mk("tensor_reduce", out=r_m1[:], in_=sel3, axis=AX.X, op=ALU.max), reads=["r_sel"], writes=["r_m1"])
        P.dve(_mk("tensor_tensor", out=r_eq[:].rearrange("p (g k) -> p g k", k=8), in0=sel3,
                  in1=r_m1[:].unsqueeze(2).to_broadcast([128, 8, 8]), op=ALU.is_equal), reads=["r_sel", "r_m1"], writes=["r_eq"])
        P.dve(_mk("scalar_tensor_tensor", out=r_sel2[:], in0=r_eq[:], scalar=-1.0e9, in1=r_sel[:], op0=ALU.mult, op1=ALU.add),
              reads=["r_eq", "r_sel"], writes=["r_sel2"])
        P.dve(_mk("tensor_reduce", out=r_m2[:], in_=r_sel2[:].rearrange("p (g k) -> p g k", k=8), axis=AX.X, op=ALU.max),
              reads=["r_sel2"], writes=["r_m2"])
        P.dve(_mk("tensor_tensor", out=r_m1[:], in0=r_m1[:], in1=r_m2[:], op=ALU.add), reads=["r_m1", "r_m2"], writes=["r_m1"])
        P.dve(_mk("max", out=r_g8[:], in_=r_m1[:]), reads=["r_m1"], writes=["r_g8"])
        P.dve(_mk("tensor_scalar", out=r_gm[:], in0=r_m1[:], scalar1=r_g8[:, 3:4], scalar2=None, op0=ALU.is_ge),
              reads=["r_m1", "r_g8"], writes=["r_gm"])
        P.dve(_mk("tensor_scalar_add", out=r_sel2[:], in0=r_sel[:], scalar1=2.0), reads=["r_sel"], writes=["r_sel2"])
        P.dve(_mk("tensor_tensor", out=r_eq[:].rearrange("p (g k) -> p g k", k=8),
                  in0=r_sel2[:].rearrange("p (g k) -> p g k", k=8),
                  in1=r_gm[:].unsqueeze(2).to_broadcast([128, 8, 8]), op=ALU.mult), reads=["r_sel2", "r_gm"], writes=["r_eq"])
        P.dve(_mk("max", out=r_e8[:], in_=r_eq[:]), reads=["r_eq"], writes=["r_e8"])
        P.dve(_mk("tensor_scalar", out=r_sel2[:], in0=r_eq[:], scalar1=r_e8[:, 7:8], scalar2=None, op0=ALU.is_ge),
              reads=["r_eq", "r_e8"], writes=["r_sel2"])
        P.dve(_mk("tensor_tensor", out=r_sel[:], in0=r_sc[:], in1=r_sel2[:], op=ALU.mult), reads=["r_sc", "r_sel2"], writes=["r_sel"])
        P.dve(_mk("tensor_reduce", out=r_ss[:], in_=r_sel[:], axis=AX.X, op=ALU.add), reads=["r_sel"], writes=["r_ss"])
        P.dve(_mk("reciprocal", out=r_ss[:], in_=r_ss[:]), reads=["r_ss"], writes=["r_ss"])
        P.dve(_mk("tensor_scalar", out=G[:, T, :], in0=r_sel[:], scalar1=r_ss[:, 0:1], scalar2=2.5, op0=ALU.mult, op1=ALU.mult),
              reads=["r_sel", "r_ss"], writes=[("G", T)])

    elist = list(range(NE)) + [NEXP]
    load_expert(elist[0])
    cnt = 0
    for ei, e in enumerate(elist):
        b = e % 2
        if ei + 1 < len(elist):
            load_expert(elist[ei + 1])
        for blk in range(NBLK):
            hb_ = hbuf[blk % 2]
            hk = ("hbuf", blk % 2)
            toks = slice(blk * 512, (blk + 1) * 512)
            tk = [("h2T", T) for T in range(blk * 4, blk * 4 + 4)]
            for ff in range(2):
                par = cnt % 2
                cnt += 1
                bg, bu = par, 2 + par
                for c in range(8):
                    P.pe(_mk("matmul", C.ps[bg][:], lhsT=wgb[b][:, c, ff * 128:(ff + 1) * 128], rhs=h2T[:, c, toks],
                             start=(c == 0), stop=(c == 7)), reads=tk + [("wgb", b)], writes=[("ps", bg)])
                for c in range(8):
                    P.pe(_mk("matmul", C.ps[bu][:], lhsT=wub[b][:, c, ff * 128:(ff + 1) * 128], rhs=h2T[:, c, toks],
                             start=(c == 0), stop=(c == 7)), reads=tk + [("wub", b)], writes=[("ps", bu)])
                P.act(_mk("activation", out=sgt[par][:], in_=C.ps[bg][:], func=AF.Silu), reads=[("ps", bg)], writes=[("sgt", par)])
                P.dve(_mk("tensor_tensor", out=hb_[:, ff, :], in0=C.ps[bu][:], in1=sgt[par][:], op=ALU.mult),
                      reads=[("ps", bu), ("sgt", par)], writes=[(hk, ff)])
            for t in range(4):
                T = blk * 4 + t
                for half in range(2):
                    by = 4 + (t * 2 + half) % 4
                    for ff in range(2):
                        P.pe(_mk("matmul", C.ps[by][:], lhsT=hb_[:, ff, t * 128:(t + 1) * 128],
                                 rhs=wdb[b][:, ff, half * 512:(half + 1) * 512], start=(ff == 0), stop=(ff == 1)),
                             reads=[(hk, 0), (hk, 1), ("wdb", b)], writes=[("ps", by)])
                    xs = X[:, T, half * 512:(half + 1) * 512]
                    scal = G[:, T, e:e + 1] if e < NEXP else 1.0
                    P.dve(_mk("scalar_tensor_tensor", out=xs, in0=C.ps[by][:], scalar=scal, in1=xs, op0=ALU.mult, op1=ALU.add),
                          reads=[("ps", by), ("G", T), ("X", T)], writes=[("X", T)])
    for T in range(NT):
        o = xo[T % 2]
        ok = ("xo", T % 2)
        emit_ln_tile(C, X[:, T, :], ("X", T), ln2g[:], ln2b[:], ["ln2g", "ln2b"], o[:], ok)
        P.dma("sp", d["x_out"][T * 128:(T + 1) * 128, :], o[:], reads=[ok], writes=[("x_out", T)], slot=("x_out", T % 2))


def rope_tables():
    t = np.arange(SEQ)
    rows = SEQ // 64
    row = (t // 64 - rows // 2).astype(np.float32)
    col = (t % 64 - 32).astype(np.float32)
    inv = (np.float32(10000.0) ** (-np.arange(16, dtype=np.float32) / np.float32(16))).astype(np.float32)
    ang = np.concatenate([row[:, None] * inv, col[:, None] * inv], -1).astype(np.float32)
    return np.cos(ang).astype(np.float32), np.sin(ang).astype(np.float32)


def tile_major(a):
    return np.ascontiguousarray(a.reshape(NT, 128, -1).transpose(1, 0, 2))


def rep128(v):
    return np.ascontiguousarray(np.broadcast_to(np.asarray(v, np.float32).reshape(1, -1), (128, v.size)))


PAIR_GROUPS = [[0, 1], [2, 3], [4, 5], [6, 7]]


def build_fused(nc, nlayers=DEPTH, groups=PAIR_GROUPS):
    C = Ctx()
    C.nc = nc
    C.P = P = Prog(nc)
    L = nlayers
    I = {}
    I["x_in"] = _din(nc, "x_in", [TOK, D])
    I["cT"] = _din(nc, "cT", [128, 8])
    I["w_ada"] = _din(nc, "w_ada", [L, D, 6 * D])
    I["b_ada"] = _din(nc, "b_ada", [L, 1, 6 * D])
    I["w_in"] = _din(nc, "w_in", [L, D, 3840])
    I["qg"] = _din(nc, "qg", [L, 128, 512])
    I["kg"] = _din(nc, "kg", [L, 128, 128])
    I["cos"] = _din(nc, "cos", [128, NT, 32])
    I["sin"] = _din(nc, "sin", [128, NT, 32])
    I["lng"] = _din(nc, "lng", [L, 128, 512])
    I["lnb"] = _din(nc, "lnb", [L, 128, 512])
    I["w_sp"] = _din(nc, "w_sp", [L, 8, 128, 128])
    I["bsT"] = _din(nc, "bsT", [L, 128, 4, 128])
    I["wa"] = _din(nc, "wa", [L, 512, D])
    I["wb"] = _din(nc, "wb", [L, 512, D])
    I["wo"] = _din(nc, "wo", [L, D, D])
    I["ln1g"] = _din(nc, "ln1g", [L, 128, D])
    I["ln1b"] = _din(nc, "ln1b", [L, 128, D])
    I["rw"] = _din(nc, "rw", [L, D, NEXP])
    I["rb"] = _din(nc, "rb", [L, 128, NEXP])
    I["wg"] = _din(nc, "wg", [L, NEXP, D, 256])
    I["wu"] = _din(nc, "wu", [L, NEXP, D, 256])
    I["wd"] = _din(nc, "wd", [L, NEXP, 256, D])
    I["wsg"] = _din(nc, "wsg", [L, D, 256])
    I["wsu"] = _din(nc, "wsu", [L, D, 256])
    I["wsd"] = _din(nc, "wsd", [L, 256, D])
    I["ln2g"] = _din(nc, "ln2g", [L, 128, D])
    I["ln2b"] = _din(nc, "ln2b", [L, 128, D])
    x_out = _dout(nc, "x_out", [TOK, D])

    def scratch(name, shape, dt):
        return nc.dram_tensor(name, list(shape), dt, kind="Internal").ap()

    xa = scratch("xa", [TOK, D], F32)
    xb = scratch("xb", [TOK, D], F32)
    kT_loc = [scratch("kT_loc%d" % l, [128, TOK], BF16) for l in range(L)]
    v_loc = [scratch("v_loc%d" % l, [TOK, 130], BF16) for l in range(L)]
    kT_all = [scratch("kT_all%d" % l, [256, TOK], BF16) for l in range(L)]
    v_all = [scratch("v_all%d" % l, [2 * TOK, 130], BF16) for l in range(L)]
    phase = [0]

    def newphase():
        phase[0] += 1
        C.g = "G:init%d" % phase[0]
        C.gp = "G:initp%d" % phase[0]
        C.arena_off = C.arena_base

    with contextlib.ExitStack() as stack:
        C.arena = stack.enter_context(nc.sbuf_tensor("arena", [128, ARENA_ELEMS], BF16))
        C.arena_off = 0
        C.ps = [stack.enter_context(nc.psum_tensor("ps%d" % i, [128, 512], F32)) for i in range(8)]
        emit_consts(C)
        alloc_common(C)
        alloc_normrope(C)
        C.arena_base = C.arena_off
        xcur = I["x_in"]
        for l in range(L):
            common = {"cT": I["cT"], "w_ada": I["w_ada"][l], "b_ada": I["b_ada"][l], "w_in": I["w_in"][l],
                      "cos": I["cos"], "sin": I["sin"]}
            newphase()
            d = dict(common)
            d.update({"x_in": xcur, "kg": I["kg"][l], "kT_out": kT_loc[l], "v_out": v_loc[l]})
            emit_kv(C, d)
            P.barrier()
            P.add("pool", _mk("collective_compute", "AllGather", ALU.bypass, replica_groups=groups,
                              ins=[kT_loc[l]], outs=[kT_all[l]]), dma="cc", inc=1)
            P.add("pool", _mk("collective_compute", "AllGather", ALU.bypass, replica_groups=groups,
                              ins=[v_loc[l]], outs=[v_all[l]]), dma="cc", inc=1)
            P.barrier()
            newphase()
            d = dict(common)
            d.update({"x_in": xcur, "x_out": xa, "qg": I["qg"][l], "kT_all": kT_all[l], "v_full": v_all[l],
                      "lng": I["lng"][l], "lnb": I["lnb"][l], "w_sp": I["w_sp"][l], "bsT": I["bsT"][l],
                      "wa": I["wa"][l], "wb": I["wb"][l], "wo": I["wo"][l], "ln1g": I["ln1g"][l], "ln1b": I["ln1b"][l]})
            emit_mix(C, d)
            P.barrier()
            newphase()
            xnext = x_out if l == L - 1 else xb
            d = dict(common)
            d.update({"x_in": xa, "x_out": xnext, "rw": I["rw"][l], "rb": I["rb"][l], "wg": I["wg"][l], "wu": I["wu"][l],
                      "wd": I["wd"][l], "wsg": I["wsg"][l], "wsu": I["wsu"][l], "wsd": I["wsd"][l],
                      "ln2g": I["ln2g"][l], "ln2b": I["ln2b"][l]})
            emit_moe(C, d)
            P.barrier()
            xcur = xb
        P.emit(final_all=True)
    return nc


def host_inputs(inp, nlayers=DEPTH, l0=0, x_cores=None):
    L = nlayers
    cosf, sinf = rope_tables()
    f32 = np.float32

    inp = dict(inp)
    for k in list(inp.keys()):
        if k not in ("x", "c"):
            inp[k] = np.asarray(inp[k])[l0:l0 + L]

    def rep(v):
        v = np.asarray(v, f32)[:L]
        return np.ascontiguousarray(np.broadcast_to(v[:, None, :], (L, 128, v.shape[1])))

    bs = np.asarray(inp["b_spatial"], f32)[:L]
    idx = (2 * np.arange(4)[None, :] + (np.arange(128) // 64)[:, None])
    bsT = np.ascontiguousarray(bs[:, idx, :])
    shared = {
        "w_ada": np.ascontiguousarray(inp["w_ada"][:L]),
        "b_ada": np.ascontiguousarray(inp["b_ada"][:L].reshape(L, 1, -1)),
        "w_in": np.ascontiguousarray(inp["w_in"][:L]),
        "qg": rep(np.tile(inp["q_scale"], (1, 8))),
        "kg": rep(np.tile(inp["k_scale"], (1, 2))),
        "lng": rep(inp["sgu_ln_g"]), "lnb": rep(inp["sgu_ln_b"]),
        "w_sp": np.ascontiguousarray(inp["w_spatial"][:L]),
        "bsT": bsT,
        "wa": np.ascontiguousarray(inp["w_branch_a"][:L]), "wb": np.ascontiguousarray(inp["w_branch_b"][:L]),
        "wo": np.ascontiguousarray(inp["w_out"][:L]),
        "ln1g": rep(inp["ln1_g"]), "ln1b": rep(inp["ln1_b"]),
        "rw": np.ascontiguousarray(inp["router_w"][:L]), "rb": rep(inp["router_bias"]),
        "wg": np.ascontiguousarray(inp["w_gate"][:L]), "wu": np.ascontiguousarray(inp["w_up"][:L]),
        "wd": np.ascontiguousarray(inp["w_down"][:L]),
        "wsg": np.ascontiguousarray(inp["ws_gate"][:L]), "wsu": np.ascontiguousarray(inp["ws_up"][:L]),
        "wsd": np.ascontiguousarray(inp["ws_down"][:L]),
        "ln2g": rep(inp["ln2_g"]), "ln2b": rep(inp["ln2_b"]),
    }
    maps = []
    for r in range(8):
        b, half = r // 2, r % 2
        sl = slice(half * TOK, (half + 1) * TOK)
        m = dict(shared)
        m["x_in"] = np.ascontiguousarray(inp["x"][b, sl]) if x_cores is None else x_cores[r]
        m["cT"] = np.ascontiguousarray(np.asarray(inp["c"], f32)[b].reshape(8, 128).T)
        m["cos"] = tile_major(cosf[sl])
        m["sin"] = tile_major(sinf[sl])
        maps.append(m)
    return maps


_NC = {}
LAYERS_PER_LAUNCH = 2


def kernel(**inp):
    inp = {k: np.asarray(v) for k, v in inp.items()}
    LPL = LAYERS_PER_LAUNCH
    if LPL not in _NC:
        nc = bass.Bass("TRN2", target_bir_lowering=False)
        build_fused(nc, nlayers=LPL)
        _NC[LPL] = nc
    x_cores = None
    for l0 in range(0, DEPTH, LPL):
        maps = host_inputs(inp, nlayers=LPL, l0=l0, x_cores=x_cores)
        res = run_bass_kernel_spmd(_NC[LPL], maps, core_ids=list(range(8)))
        x_cores = [np.ascontiguousarray(res.results[r]["x_out"]) for r in range(8)]
    out = np.zeros((4, SEQ, D), np.float32)
    for r in range(8):
        out[r // 2, (r % 2) * TOK:(r % 2 + 1) * TOK] = x_cores[r]
    return out
```

```python
import contextlib
import os
import numpy as np
import ml_dtypes
import concourse.bass as bass
import concourse.mybir as mybir
from concourse.bass_utils import run_bass_kernel_spmd

F32 = mybir.dt.float32
BF16 = mybir.dt.bfloat16
AF = mybir.ActivationFunctionType
ALU = mybir.AluOpType
AX = mybir.AxisListType

D = 1024
SEQ = 4096
TOK = 2048
NT = TOK // 128
NBLK = 4
DEPTH = 4
ALPHA = (2 * DEPTH) ** 0.25
NEXP = 64
ENGS = ("pe", "act", "dve", "pool", "sp")
SEM_LIMIT = 15000
FUSE_WAITS = False


class Op:
    __slots__ = ("eng", "fn", "deps", "users", "sem", "val", "dma", "waits", "inc")

    def __init__(self, eng, fn, dma):
        self.eng = eng
        self.fn = fn
        self.deps = []
        self.users = 0
        self.sem = None
        self.val = 0
        self.dma = dma
        self.waits = []


class Prog:
    def __init__(self, nc):
        self.nc = nc
        self.ops = {e: [] for e in ENGS}
        self.order = []
        self.lastw = {}
        self.readers = {}
        self.last_dma = {}
        self.pending = {}

    def barrier_list(self):
        lasts = [self.ops[e][-1] for e in ENGS if self.ops[e]]
        lasts.extend(self.last_dma.values())
        return lasts

    def barrier(self):
        lasts = self.barrier_list()
        self.pending = {e: list(lasts) for e in ENGS}
        self.lastw.clear()
        self.readers.clear()

    def add(self, eng, fn, reads=(), writes=(), dma=None, inc=None):
        op = Op(eng, fn, dma)
        op.inc = inc if inc is not None else (16 if dma is not None else 1)
        deps = list(self.pending.pop(eng, ()))
        for k in reads:
            w = self.lastw.get(k)
            if w is not None:
                deps.append(w)
        for k in writes:
            w = self.lastw.get(k)
            if w is not None:
                deps.append(w)
            deps.extend(self.readers.get(k, ()))
        seen = set()
        for d in deps:
            if id(d) in seen or d is op:
                continue
            seen.add(id(d))
            if d.eng == "pe" and eng == "pe" and d.dma is None and dma is None:
                continue
            op.deps.append(d)
            d.users += 1
        for k in reads:
            self.readers.setdefault(k, []).append(op)
        for k in writes:
            self.lastw[k] = op
            self.readers[k] = []
        self.ops[eng].append(op)
        self.order.append(op)
        if dma is not None:
            self.last_dma[dma] = op
        return op

    def pe(self, fn, reads=(), writes=()):
        return self.add("pe", fn, reads, writes)

    def act(self, fn, reads=(), writes=()):
        return self.add("act", fn, reads, writes)

    def dve(self, fn, reads=(), writes=()):
        return self.add("dve", fn, reads, writes)

    def pool(self, fn, reads=(), writes=()):
        return self.add("pool", fn, reads, writes)

    def dma(self, eng, out, in_, reads=(), writes=(), slot=None):
        assert slot is not None
        return self.add(eng, _mk("dma_start", out=out, in_=in_), reads, writes, dma=slot)

    def emit(self, final_keys=(), final_all=False):
        nc = self.nc
        fin = Op("sp", None, None)
        for k in final_keys:
            w = self.lastw.get(k)
            if w is not None:
                fin.deps.append(w)
                w.users += 1
        if final_all:
            for w in self.barrier_list():
                fin.deps.append(w)
                w.users += 1
        sem_ctx = []
        eng_sems = {e: [] for e in ENGS}
        counters = {e: 0 for e in ENGS}
        slot_cnt = {}
        slot_sem = {}
        nsem = [0]

        def new_sem():
            cm = nc.semaphore("sm%d" % nsem[0])
            s = cm.__enter__()
            sem_ctx.append(cm)
            nsem[0] += 1
            return s

        for op in self.order:
            if op.dma is not None:
                if op.dma not in slot_sem or slot_cnt[op.dma] + op.inc > SEM_LIMIT:
                    slot_sem[op.dma] = new_sem()
                    slot_cnt[op.dma] = 0
                slot_cnt[op.dma] += op.inc
                op.sem = slot_sem[op.dma]
                op.val = slot_cnt[op.dma]
            elif op.users > 0:
                e = op.eng
                if not eng_sems[e] or counters[e] + 1 > SEM_LIMIT:
                    eng_sems[e].append(new_sem())
                    counters[e] = 0
                counters[e] += 1
                op.sem = eng_sems[e][-1]
                op.val = counters[e]
        for op in self.order:
            if op.dma is not None and isinstance(op.dma, str) and op.dma.startswith("G:"):
                assert slot_cnt[op.dma] <= SEM_LIMIT
                op.val = slot_cnt[op.dma]
        if os.environ.get("PROG_DEBUG"):
            names = {}
            for op in self.order:
                if op.sem is not None:
                    names.setdefault(id(op.sem), (op.dma if op.dma is not None else op.eng, []))[1].append(op.val)
            for i, (k, (nm, vals)) in enumerate(names.items()):
                print("SEM", i, nm, "n=%d max=%d" % (len(vals), max(vals)))
        waited = {e: {} for e in ENGS}

        def calc_waits(op):
            ws = {}
            for d in op.deps:
                key = id(d.sem)
                if waited[op.eng].get(key, 0) >= d.val:
                    continue
                if key not in ws or ws[key][1] < d.val:
                    ws[key] = (d.sem, d.val)
            for key, (s, v) in ws.items():
                waited[op.eng][key] = v
            return list(ws.values())

        for op in self.order:
            op.waits = calc_waits(op)
        fin.waits = calc_waits(fin)
        self.n_sems = nsem[0]
        engmap = {"pe": "tensor", "act": "scalar", "dve": "vector", "pool": "gpsimd", "sp": "sync"}
        stats = [0]
        self.stats = stats
        with nc.Block() as block:
            for e in ENGS:
                ops = self.ops[e]

                def body(eng, ops=ops, is_last=(e == "sp")):
                    for op in ops:
                        ws = list(op.waits)
                        fused = None
                        if ws and op.dma is None and FUSE_WAITS:
                            fused = ws.pop()
                        for s, v in ws:
                            eng.wait_ge(s, v)
                        ins = op.fn(eng)
                        if fused is not None:
                            ins._wait_ge(fused[0], fused[1])
                        if op.sem is not None:
                            ins.then_inc(op.sem, op.inc)
                        stats[0] += 1 + len(ws)
                    if is_last:
                        for s, v in fin.waits:
                            eng.wait_ge(s, v)

                getattr(block, engmap[e])(body)
        for cm in reversed(sem_ctx):
            cm.__exit__(None, None, None)


class Ctx:
    pass


def _mk(method, *a, **k):
    return lambda e: getattr(e, method)(*a, **k)


def _din(nc, name, shape, dt=F32):
    return nc.dram_tensor(name, list(shape), dt, kind="ExternalInput").ap()


def _dout(nc, name, shape, dt=F32):
    return nc.dram_tensor(name, list(shape), dt, kind="ExternalOutput").ap()


_DTSIZE = {F32: 4, BF16: 2}
ARENA_ELEMS = 106000


def _sb(C, name, shape, dt=F32):
    n = 1
    for v in shape[1:]:
        n *= v
    nbytes = n * _DTSIZE[dt]
    off = (C.arena_off + 63) // 64 * 64
    assert off + nbytes <= ARENA_ELEMS * 2, ("SBUF arena overflow", name, off, nbytes)
    C.arena_off = off + nbytes
    C.arena_peak = max(getattr(C, "arena_peak", 0), C.arena_off)
    ap = C.arena[0:shape[0], off // 2:(off + nbytes) // 2]
    if dt != BF16:
        ap = ap.bitcast(dt)
    if len(shape) == 3:
        ap = ap.rearrange("p (a b) -> p a b", a=shape[1], b=shape[2])
    elif len(shape) != 2:
        raise ValueError(shape)
    return ap


def emit_consts(C):
    P = C.P
    C.ident_b = _sb(C, "ident_b", [128, 128], BF16)
    C.ident_f = _sb(C, "ident_f", [128, 128], F32)
    C.ones_f = _sb(C, "ones_f", [128, 128], F32)
    C.mhalf = _sb(C, "mhalf", [128, 16], F32)
    for t, k in ((C.ident_b, "ident_b"), (C.ident_f, "ident_f")):
        P.pool(_mk("memset", t[:], 0.0), writes=[k])
        P.pool(_mk("affine_select", out=t[:], in_=t[:], pattern=[[-1, 128]],
                                             compare_op=ALU.not_equal, fill=1.0, base=0,
                                             channel_multiplier=1), reads=[k], writes=[k])
    P.pool(_mk("memset", C.ones_f[:], 1.0), writes=["ones_f"])
    P.pool(_mk("memset", C.mhalf[:], -0.5), writes=["mhalf"])


def emit_mod(C, col0, nslab, dst, dkey, wbuf, wkeys):
    P, nc = C.P, C.nc
    cT = _sb(C, "cT_sb", [128, 8], F32)
    cond = _sb(C, "cond_sb", [128, 8], F32)
    lhs = _sb(C, "modlhs", [128, 8, 128], F32)
    bbuf = [_sb(C, "modb%d" % i, [1, 512], F32) for i in range(2)]
    P.dma("sp", cT[:], C.d["cT"], writes=["cT"], slot="cT")
    P.act(_mk("activation", out=cond[:], in_=cT[:], func=AF.Silu), reads=["cT"], writes=["cond"])
    P.dve(_mk("tensor_copy", out=lhs[:], in_=cond[:].unsqueeze(2).to_broadcast([128, 8, 128])),
          reads=["cond"], writes=["modlhs"])
    for s in range(nslab):
        b = s % 2
        c0 = col0 + s * 512
        P.dma("sp", wbuf[b], C.d["w_ada"][:, c0:c0 + 512].rearrange("(c p) n -> p c n", p=128),
              writes=list(wkeys[b]), slot=("modw", b))
        P.dma("sp", bbuf[b][:], C.d["b_ada"][0:1, c0:c0 + 512], writes=[("modb", b)], slot=("modb", b))
        pk = ("ps", s % 2)
        ps = C.ps[s % 2]
        for c in range(8):
            P.pe(_mk("matmul", ps[:], lhsT=lhs[:, c, :], rhs=wbuf[b][:, c, :],
                                                   start=(c == 0), stop=False),
                 reads=["modlhs"] + list(wkeys[b]), writes=[pk])
        P.pe(_mk("matmul", ps[:], lhsT=C.ones_f[0:1, :], rhs=bbuf[b][0:1, :],
                                            start=False, stop=True),
             reads=["ones_f", ("modb", b)], writes=[pk])
        P.act(_mk("copy", out=dst[:, s * 512:(s + 1) * 512], in_=ps[:]),
              reads=[pk], writes=[dkey])


def emit_ln_tile(C, xr, xrkey, g, b, gbkey, out, outkey, eps=1e-5):
    P = C.P
    st, mv, rs = C.ln_st, C.ln_mv, C.ln_rs
    for i in range(2):
        P.dve(_mk("bn_stats", out=st[:, i, :], in_=xr[:, i * 512:(i + 1) * 512]),
              reads=[xrkey], writes=[("ln_st", i)])
    P.dve(_mk("bn_aggr", out=mv[:], in_=st[:]), reads=[("ln_st", 0), ("ln_st", 1)], writes=["ln_mv"])
    P.dve(_mk("tensor_scalar_add", out=rs[:], in0=mv[:, 1:2], scalar1=eps), reads=["ln_mv"], writes=["ln_rs"])
    P.pool(_mk("tensor_tensor", out=rs[:], in0=rs[:], in1=C.mhalf[:, 0:1], op=ALU.pow),
           reads=["ln_rs", "mhalf"], writes=["ln_rs"])
    P.dve(_mk("tensor_scalar", out=xr, in0=xr, scalar1=mv[:, 0:1], scalar2=rs[:, 0:1],
                                    op0=ALU.subtract, op1=ALU.mult),
          reads=[xrkey, "ln_mv", "ln_rs"], writes=[xrkey])
    P.pool(_mk("tensor_tensor", out=xr, in0=xr, in1=g, op=ALU.mult), reads=[xrkey] + list(gbkey), writes=[xrkey])
    P.pool(_mk("tensor_tensor", out=out, in0=xr, in1=b, op=ALU.add), reads=[xrkey] + list(gbkey), writes=[outkey])


def emit_normrope(C, ps_ap, pskey, hi, lo, gsb, gkey, cs, sn, cskey, out_view, outkey):
    P = C.P
    nh = hi * lo
    W = nh * 64
    sq, sqk = C.F[0], ("F", 0)
    qn, qnk = C.F[1], ("F", 1)
    ss = C.nr_ss
    tA, tB, tC, tD = C.nr_t
    P.act(_mk("activation", out=sq[:, 0:W], in_=ps_ap, func=AF.Square), reads=[pskey], writes=[sqk])
    P.dve(_mk("tensor_reduce", out=ss[:, 0:nh], in_=sq[:, 0:W].rearrange("p (h d) -> p h d", d=64),
                                    axis=AX.X, op=ALU.add), reads=[sqk], writes=["nr_ss"])
    P.dve(_mk("tensor_scalar", out=ss[:, 0:nh], in0=ss[:, 0:nh], scalar1=1.0 / 64, scalar2=1e-6,
                                    op0=ALU.mult, op1=ALU.add), reads=["nr_ss"], writes=["nr_ss"])
    P.pool(_mk("tensor_tensor", out=ss[:, 0:nh], in0=ss[:, 0:nh], in1=C.mhalf[:, 0:nh], op=ALU.pow),
           reads=["nr_ss", "mhalf"], writes=["nr_ss"])
    P.dve(_mk("tensor_tensor", out=qn[:, 0:W].rearrange("p (h d) -> p h d", d=64),
                                    in0=ps_ap.rearrange("p (h d) -> p h d", d=64),
                                    in1=ss[:, 0:nh].unsqueeze(2).to_broadcast([128, nh, 64]), op=ALU.mult),
          reads=[pskey, "nr_ss"], writes=[qnk])
    P.pool(_mk("tensor_tensor", out=qn[:, 0:W], in0=qn[:, 0:W], in1=gsb, op=ALU.mult),
           reads=[qnk, gkey], writes=[qnk])
    qv = qn[:, 0:W].rearrange("p (a b i two) -> p a b i two", a=hi, b=lo, two=2)
    x0 = qv[:, :, :, :, 0]
    x1 = qv[:, :, :, :, 1]
    cb = cs.unsqueeze(1).unsqueeze(1).to_broadcast([128, hi, lo, 32])
    sb_ = sn.unsqueeze(1).unsqueeze(1).to_broadcast([128, hi, lo, 32])

    def v4(t):
        return t[:, 0:nh * 32].rearrange("p (a b i) -> p a b i", a=hi, b=lo)

    P.dve(_mk("tensor_tensor", out=v4(tA), in0=x0, in1=cb, op=ALU.mult), reads=[qnk] + list(cskey), writes=["nr_tA"])
    P.dve(_mk("tensor_tensor", out=v4(tB), in0=x1, in1=sb_, op=ALU.mult), reads=[qnk] + list(cskey), writes=["nr_tB"])
    P.dve(_mk("tensor_tensor", out=out_view[:, :, :, :, 0], in0=v4(tA), in1=v4(tB), op=ALU.subtract),
          reads=["nr_tA", "nr_tB"], writes=[outkey + "_e"])
    P.pool(_mk("tensor_tensor", out=v4(tC), in0=x0, in1=sb_, op=ALU.mult), reads=[qnk] + list(cskey), writes=["nr_tC"])
    P.pool(_mk("tensor_tensor", out=v4(tD), in0=x1, in1=cb, op=ALU.mult), reads=[qnk] + list(cskey), writes=["nr_tD"])
    P.pool(_mk("tensor_tensor", out=out_view[:, :, :, :, 1], in0=v4(tC), in1=v4(tD), op=ALU.add),
           reads=["nr_tC", "nr_tD"], writes=[outkey + "_o"])


def alloc_common(C):
    C.hf = _sb(C, "hf", [128, 1024], F32)
    C.hb = _sb(C, "hb", [128, 1024], BF16)
    C.ln_st = _sb(C, "ln_st", [128, 2, 6], F32)
    C.ln_mv = _sb(C, "ln_mv", [128, 2], F32)
    C.ln_rs = _sb(C, "ln_rs", [128, 1], F32)
    C.F = [_sb(C, "F%d" % i, [128, 512], F32) for i in range(4)]


def alloc_normrope(C):
    C.nr_ss = _sb(C, "nr_ss", [128, 8], F32)
    C.nr_t = [_sb(C, "nr_t%d" % i, [128, 256], F32) for i in range(4)]


def emit_hT_block(C, blk, xsrc, sc1p, sh1, hT):
    P = C.P
    for t in range(4):
        T = blk * 4 + t
        xt, xkey = xsrc(T)
        P.dve(_mk("tensor_tensor", out=C.hf[:], in0=xt, in1=sc1p[:], op=ALU.mult),
              reads=[xkey, "mod1"], writes=["hf"])
        P.pool(_mk("tensor_tensor", out=C.hb[:], in0=C.hf[:], in1=sh1[:], op=ALU.add),
               reads=["hf", "mod1"], writes=["hb"])
        pb = t % 2
        pk = ("ps", pb)
        psv = C.ps[pb][:].bitcast(BF16)
        for c in range(8):
            P.pe(_mk("transpose", out=psv[:, c * 128:(c + 1) * 128],
                                                     in_=C.hb[:, c * 128:(c + 1) * 128], identity=C.ident_b[:]),
                 reads=["hb", "ident_b"], writes=[pk])
        P.act(_mk("copy", out=hT[:, :, t * 128:(t + 1) * 128],
                                             in_=psv.rearrange("p (c k) -> p c k", c=8)),
              reads=[pk], writes=[("hT", t)])


def emit_kv(C, d):
    P = C.P
    nc = C.nc
    C.d = d
    mod = _sb(C, "mod1", [128, 2048], F32)
    mw = [_sb(C, "modw%d" % i, [128, 8, 512], F32) for i in range(2)]
    sh1 = mod[:, 0:1024]
    sc1p = mod[:, 1024:2048]
    wkv = _sb(C, "wkv", [128, 8, 256], BF16)
    P.dma("pool", wkv[:], d["w_in"][:, 512:768].rearrange("(c p) n -> p c n", p=128), writes=["wkv"], slot=C.gp)
    kg = _sb(C, "kg", [128, 128], F32)
    P.dma("sp", kg[:], d["kg"], writes=["kg"], slot=C.g)
    cs = _sb(C, "cos", [128, NT, 32], F32)
    sn = _sb(C, "sin", [128, NT, 32], F32)
    P.dma("sp", cs[:], d["cos"], writes=["cs"], slot=C.g)
    P.dma("sp", sn[:], d["sin"], writes=["sn"], slot=C.g)
    emit_mod(C, 0, 4, mod, "mod1", [mw[0][:], mw[1][:]], [[("modw", 0)], [("modw", 1)]])
    P.dve(_mk("tensor_scalar_add", out=sc1p, in0=sc1p, scalar1=1.0), reads=["mod1"], writes=["mod1"])
    xt = [_sb(C, "xt%d" % i, [128, 1024], F32) for i in range(2)]
    hT = _sb(C, "hT", [128, 8, 512], BF16)
    kr = _sb(C, "kr", [128, 128], BF16)
    kT = _sb(C, "kT", [128, TOK], BF16)
    va = _sb(C, "va", [128, NT, 130], BF16)
    P.pool(_mk("memset", va[:], 1.0), writes=["va"])

    def xsrc(T):
        b = T % 2
        P.dma("sp", xt[b][:], d["x_in"][T * 128:(T + 1) * 128, :], writes=[("xt", b)], slot=("xt", b))
        return xt[b][:], ("xt", b)

    for blk in range(NBLK):
        emit_hT_block(C, blk, xsrc, sc1p, sh1, hT)
        for t in range(4):
            T = blk * 4 + t
            pb = 2 + t % 2
            pk = ("ps", pb)
            ps = C.ps[pb]
            for c in range(8):
                P.pe(_mk("matmul", ps[:, 0:256], lhsT=hT[:, c, t * 128:(t + 1) * 128],
                                                       rhs=wkv[:, c, :], start=(c == 0), stop=(c == 7)),
                     reads=[("hT", t), "wkv"], writes=[pk])
            P.act(_mk("copy", out=va[:, T, :].rearrange("p (h d) -> p h d", h=2)[:, :, 0:64],
                                               in_=ps[:, 128:256].rearrange("p (h d) -> p h d", h=2)),
                  reads=[pk, "va"], writes=["va"])
            emit_normrope(C, ps[:, 0:128], pk, 2, 1, kg[:], "kg", cs[:, T, :], sn[:, T, :], ["cs", "sn"],
                          kr[:].rearrange("p (a b i two) -> p a b i two", a=2, b=1, two=2), "kr")
            pb2 = 4 + t % 2
            pk2 = ("ps", pb2)
            psv = C.ps[pb2][:].bitcast(BF16)
            P.pe(_mk("transpose", out=psv[:, 0:128], in_=kr[:], identity=C.ident_b[:]),
                 reads=["kr_e", "kr_o", "ident_b"], writes=[pk2])
            P.act(_mk("copy", out=kT[:, T * 128:(T + 1) * 128], in_=psv[:, 0:128]),
                  reads=[pk2], writes=["kT"])
    P.dma("sp", d["kT_out"], kT[:], reads=["kT"], writes=["kT_out"], slot="kT_out")
    P.dma("sp", d["v_out"].rearrange("(t p) f -> p t f", p=128), va[:], reads=["va"], writes=["v_out"], slot="v_out")


def emit_mix(C, d):
    P = C.P
    nc = C.nc
    C.d = d
    NR = 6
    ring = _sb(C, "ring", [128, NR * 4096], BF16)
    mod = _sb(C, "mod1", [128, 3072], F32)
    mw = [ring[:, b * 8192:(b + 1) * 8192].bitcast(F32).rearrange("p (c n) -> p c n", c=8) for b in range(2)]
    sh1 = mod[:, 0:1024]
    sc1p = mod[:, 1024:2048]
    gate1 = mod[:, 2048:3072]

    def small(name, shape, dt=F32, src=None, eng="sp"):
        t = _sb(C, name, shape, dt)
        P.dma(eng, t[:], d[name] if src is None else src, writes=[name], slot=C.g if eng == "sp" else C.gp)
        return t

    qg = small("qg", [128, 512])
    cs = small("cos", [128, NT, 32])
    sn = small("sin", [128, NT, 32])
    lng = small("lng", [128, 512])
    lnb = small("lnb", [128, 512])
    bsT = small("bsT", [128, 4, 128])
    ln1g = small("ln1g", [128, D])
    ln1b = small("ln1b", [128, D])
    kT = _sb(C, "kT_full", [128, SEQ], BF16)
    for r in range(2):
        P.dma("sp", kT[:, r * TOK:(r + 1) * TOK], d["kT_all"][r * 128:(r + 1) * 128, :], writes=[("kT_full", r)], slot=C.g)
    vsb = _sb(C, "v_full", [128, 32, 130], BF16)
    for i in range(4):
        P.dma("sp", vsb[:, i * 8:(i + 1) * 8, :],
              d["v_full"][i * 1024:(i + 1) * 1024, :].rearrange("(t p) f -> p t f", p=128),
              writes=[("v_full", i)], slot=C.g)
    ones_b = _sb(C, "ones_b", [128, 64], BF16)
    P.pool(_mk("memset", ones_b[:], 1.0), writes=["ones_b"])
    emit_mod(C, 0, 6, mod, "mod1", mw, [[("slab", 0), ("slab", 1)], [("slab", 2), ("slab", 3)]])
    P.dve(_mk("tensor_scalar_add", out=sc1p, in0=sc1p, scalar1=1.0), reads=["mod1"], writes=["mod1"])
    wsp = small("w_sp", [128, 8, 128], BF16, src=d["w_sp"].rearrange("g i j -> i g j"), eng="pool")
    wsT = _sb(C, "wsT", [128, 8, 128], BF16)
    psv = C.ps[2][:].bitcast(BF16)
    for g in range(8):
        P.pe(_mk("transpose", out=psv[:, g * 128:(g + 1) * 128], in_=wsp[:, g, :], identity=C.ident_b[:]),
             reads=["w_sp", "ident_b"], writes=[("ps", 2)])
    P.act(_mk("copy", out=wsT[:].rearrange("p g i -> p (g i)"), in_=psv), reads=[("ps", 2)], writes=["wsT"])

    q_pad = _sb(C, "q_pad", [128, 8, 512], BF16)
    P.pool(_mk("memset", q_pad[:], 0.0), writes=["q_pad"])
    NXT = 4
    xt = [_sb(C, "xt%d" % i, [128, 1024], F32) for i in range(NXT)]
    hT = _sb(C, "hT", [128, 8, 512], BF16)
    q_r = _sb(C, "q_r", [128, 512], BF16)
    uT = _sb(C, "uT", [128, 4, 512], BF16)
    vnb = _sb(C, "vnb", [128, 512], BF16)
    o_bT = _sb(C, "o_bT", [128, 4, 512], BF16)
    pT = [_sb(C, "pT%d" % i, [128, 512], BF16) for i in range(3)]
    o_aT = _sb(C, "o_aT", [128, 8, 512], BF16)
    sg = [[_sb(C, "sg%d%d" % (i, j), [128, 512], BF16) for j in range(2)] for i in range(2)]
    yT = _sb(C, "yT", [128, 8, 512], BF16)
    xo = _sb(C, "xo", [128, 1024], F32)
    sg_st = _sb(C, "sg_st", [128, 6], F32)
    sg_mv = _sb(C, "sg_mv", [128, 2], F32)
    sg_rs = _sb(C, "sg_rs", [128, 1], F32)

    def cp(ap):
        return ap.rearrange("(c p) n -> p c n", p=128)

    w_in = d["w_in"]
    specs = [
        ("q", cp(w_in[:, 0:512]), 128, 8),
        ("zv", cp(w_in[:, 1280:1792]), 128, 8),
        ("zu", cp(w_in[:, 768:1280]), 128, 8),
        ("wa0", d["wa"][:, 0:512].rearrange("(h k) n -> k h n", k=64), 64, 8),
        ("wb", cp(d["wb"]), 128, 4),
        ("ga0", cp(w_in[:, 1792:2304]), 128, 8),
        ("gb0", cp(w_in[:, 2816:3328]), 128, 8),
        ("wa1", d["wa"][:, 512:1024].rearrange("(h k) n -> k h n", k=64), 64, 8),
        ("ga1", cp(w_in[:, 2304:2816]), 128, 8),
        ("gb1", cp(w_in[:, 3328:3840]), 128, 8),
        ("wo0", cp(d["wo"][:, 0:512]), 128, 8),
        ("wo1", cp(d["wo"][:, 512:1024]), 128, 8),
    ]
    NS = len(specs)
    total = NBLK * NS
    state = {"next": 0}

    def slab_view(gi):
        name, src, npart, c = specs[gi % NS]
        b = gi % NR
        return ring[0:npart, b * 4096:(b + 1) * 4096].rearrange("p (c n) -> p c n", c=c), ("slab", b)

    def ensure(gi):
        while state["next"] <= min(gi, total - 1):
            g = state["next"]
            name, src, npart, c = specs[g % NS]
            v, key = slab_view(g)
            P.dma("pool", v, src, writes=[key], slot=key)
            state["next"] += 1

    def slab(blk, j):
        gi = blk * NS + j
        assert gi < state["next"], (gi, state["next"])
        return slab_view(gi)

    def stage(blk, lo):
        ensure(blk * NS + lo + NR - 1)

    def xsrc(T):
        b = T % NXT
        P.dma("sp", xt[b][:], d["x_in"][T * 128:(T + 1) * 128, :], writes=[("xt", b)], slot=("xt", b))
        return xt[b][:], ("xt", b)

    hTk = [("hT", t) for t in range(4)]
    STOP = int(os.environ.get("MIX_STOP", "99"))
    for blk in range(NBLK if STOP > 0 else 0):
        emit_hT_block(C, blk, xsrc, sc1p, sh1, hT)
        if STOP <= 1:
            break
        stage(blk, 0)
        wq, wqk = slab(blk, 0)
        for t in range(4):
            T = blk * 4 + t
            pb = 2 + t % 2
            pk = ("ps", pb)
            ps = C.ps[pb]
            for c in range(8):
                P.pe(_mk("matmul", ps[:], lhsT=hT[:, c, t * 128:(t + 1) * 128], rhs=wq[:, c, :],
                                                       start=(c == 0), stop=(c == 7)),
                     reads=[("hT", t), wqk], writes=[pk])
            emit_normrope(C, ps[:], pk, 2, 4, qg[:], "qg", cs[:, T, :], sn[:, T, :], ["cos", "sin"],
                          q_r[:].rearrange("p (b a i two) -> p a b i two", a=2, b=4, two=2), "q_r")
            pb2 = 4 + t % 2
            pk2 = ("ps", pb2)
            pv = C.ps[pb2][:].bitcast(BF16)
            for j in range(4):
                P.pe(_mk("transpose", out=pv[:, j * 128:(j + 1) * 128], in_=q_r[:, j * 128:(j + 1) * 128],
                                                       identity=C.ident_b[:]),
                     reads=["q_r_e", "q_r_o", "ident_b"], writes=[pk2])
            pv3 = pv[:, 0:512].rearrange("p (j k) -> p j k", j=4)
            P.act(_mk("copy", out=q_pad[0:64, 0:4, t * 128:(t + 1) * 128], in_=pv3[0:64]),
                  reads=[pk2], writes=[("q_pad", 0, t)])
            P.dve(_mk("tensor_copy", out=q_pad[64:128, 4:8, t * 128:(t + 1) * 128], in_=pv3[64:128]),
                  reads=[pk2], writes=[("q_pad", 1, t)])
        if STOP <= 2:
            break
        qpk = ["q_pad"] + [("q_pad", i, t) for i in range(2) for t in range(4)]
        stage(blk, 1)
        wzv, wzvk = slab(blk, 1)
        wzu, wzuk = slab(blk, 2)
        for cc in range(4):
            pb = 6 + cc % 2
            pk = ("ps", pb)
            ps = C.ps[pb]
            for c in range(8):
                P.pe(_mk("matmul", ps[:], lhsT=wzu[:, c, cc * 128:(cc + 1) * 128], rhs=hT[:, c, :],
                                                         start=(c == 0), stop=(c == 7)),
                     reads=hTk + [wzuk], writes=[pk])
            P.act(_mk("activation", out=uT[:, cc, :], in_=ps[:], func=AF.Gelu),
                  reads=[pk], writes=[("uT", cc)])
        uTk = [("uT", cc) for cc in range(4)]
        vg, vgk = C.F[2], ("F", 2)
        stmp, stmpk = C.F[3], ("F", 3)
        for t in range(4):
            pb = 2 + t % 2
            pk = ("ps", pb)
            ps = C.ps[pb]
            for c in range(8):
                P.pe(_mk("matmul", ps[:], lhsT=hT[:, c, t * 128:(t + 1) * 128], rhs=wzv[:, c, :],
                                                       start=(c == 0), stop=(c == 7)),
                     reads=[("hT", t), wzvk], writes=[pk])
            P.act(_mk("activation", out=vg[:], in_=ps[:], func=AF.Gelu), reads=[pk], writes=[vgk])
            P.dve(_mk("bn_stats", out=sg_st[:], in_=vg[:]), reads=[vgk], writes=["sg_st"])
            P.dve(_mk("bn_aggr", out=sg_mv[:], in_=sg_st[:]), reads=["sg_st"], writes=["sg_mv"])
            P.dve(_mk("tensor_scalar_add", out=sg_rs[:], in0=sg_mv[:, 1:2], scalar1=1e-5), reads=["sg_mv"], writes=["sg_rs"])
            P.pool(_mk("tensor_tensor", out=sg_rs[:], in0=sg_rs[:], in1=C.mhalf[:, 0:1], op=ALU.pow),
                   reads=["sg_rs", "mhalf"], writes=["sg_rs"])
            P.dve(_mk("tensor_scalar", out=vg[:], in0=vg[:], scalar1=sg_mv[:, 0:1], scalar2=sg_rs[:, 0:1],
                                            op0=ALU.subtract, op1=ALU.mult), reads=[vgk, "sg_mv", "sg_rs"], writes=[vgk])
            P.pool(_mk("tensor_tensor", out=vg[:], in0=vg[:], in1=lng[:], op=ALU.mult), reads=[vgk, "lng"], writes=[vgk])
            P.pool(_mk("tensor_tensor", out=vnb[:], in0=vg[:], in1=lnb[:], op=ALU.add), reads=[vgk, "lnb"], writes=["vnb"])
            pb2 = 4 + t % 2
            pk2 = ("ps", pb2)
            ps2 = C.ps[pb2]
            for g in range(8):
                P.pe(_mk("matmul", ps2[(g % 2) * 64:(g % 2) * 64 + 64, (g // 2) * 128:(g // 2) * 128 + 128],
                                                      lhsT=vnb[:, g * 64:(g + 1) * 64], rhs=wsT[:, g, :],
                                                      start=True, stop=True),
                     reads=["vnb", "wsT"], writes=[pk2])
            P.dve(_mk("tensor_tensor", out=stmp[:].rearrange("p (c i) -> p c i", c=4),
                                                     in0=ps2[:].rearrange("p (c i) -> p c i", c=4), in1=bsT[:], op=ALU.add),
                  reads=[pk2, "bsT"], writes=[stmpk])
            P.dve(_mk("tensor_tensor", out=o_bT[:, :, t * 128:(t + 1) * 128],
                                                 in0=stmp[:].rearrange("p (c i) -> p c i", c=4),
                                                 in1=uT[:, :, t * 128:(t + 1) * 128], op=ALU.mult),
                  reads=[stmpk] + uTk, writes=[("o_bT", t)])
        obk = [("o_bT", t) for t in range(4)]
        if STOP <= 3:
            break
        rden, rdenk = C.F[3], ("F", 3)
        stage(blk, 3)
        pending = [None]

        def tail(h, accb):
            acc = C.ps[accb]
            ak = ("ps", accb)
            den = C.ps[accb + 2]
            dk = ("ps", accb + 2)
            P.dve(_mk("reciprocal", out=rden[0:64, :], in_=den[0:64, :]), reads=[dk], writes=[rdenk])
            P.dve(_mk("tensor_tensor", out=o_aT[0:64, h, :], in0=acc[0:64, :], in1=rden[0:64, :], op=ALU.mult),
                  reads=[ak, rdenk], writes=[("o_aT", h)])

        for h in range(8):
            kvh = h // 4
            accb = 3 + h % 2
            acc = C.ps[accb]
            ak = ("ps", accb)

            def s_mm(kt, h=h):
                sbk = kt % 3
                P.pe(_mk("matmul", C.ps[sbk][:], lhsT=kT[:, kt * 128:(kt + 1) * 128], rhs=q_pad[:, h, :],
                                        start=True, stop=True),
                     reads=[("kT_full", 0), ("kT_full", 1)] + qpk, writes=[("ps", sbk)])
                P.act(_mk("activation", out=pT[sbk][:], in_=C.ps[sbk][:], func=AF.Exp, scale=0.125),
                      reads=[("ps", sbk)], writes=[("pT", sbk)])

            def pv_mm(kt, kvh=kvh, acc=acc, ak=ak, accb=accb):
                sbk = kt % 3
                P.pe(_mk("matmul", acc[0:64, :], lhsT=vsb[:, kt, kvh * 65:kvh * 65 + 64], rhs=pT[sbk][:],
                                        start=(kt == 0), stop=(kt == 31)),
                     reads=[("pT", sbk)] + [("v_full", i) for i in range(4)], writes=[ak])
                P.pe(_mk("matmul", C.ps[accb + 2][0:64, :], lhsT=ones_b[:, :], rhs=pT[sbk][:],
                                        start=(kt == 0), stop=(kt == 31)),
                     reads=[("pT", sbk), "ones_b"], writes=[("ps", accb + 2)])

            s_mm(0)
            s_mm(1)
            if pending[0] is not None:
                tail(*pending[0])
            for kt in range(32):
                if kt + 2 < 32:
                    s_mm(kt + 2)
                pv_mm(kt)
            pending[0] = (h, accb)
        tail(*pending[0])
        oak = [("o_aT", h) for h in range(8)]
        if STOP <= 4:
            break
        t1, t1k = C.F[0], ("F", 0)
        t2, t2k = C.F[1], ("F", 1)
        for m in range(8):
            half = m // 4
            if m == 4:
                stage(blk, 4)
            mm = m % 4
            wa_s, wak = slab(blk, 3 if half == 0 else 7)
            wb_s, wbk = slab(blk, 4)
            ga_s, gak = slab(blk, 5 if half == 0 else 8)
            gb_s, gbk = slab(blk, 6 if half == 0 else 9)
            par = m % 2
            b_ya, b_ga, b_yb, b_gb = [4 * par + i for i in range(4)]
            for h in range(8):
                P.pe(_mk("matmul", C.ps[b_ya][:], lhsT=wa_s[0:64, h, mm * 128:(mm + 1) * 128], rhs=o_aT[0:64, h, :],
                                             start=(h == 0), stop=(h == 7)),
                     reads=oak + [wak], writes=[("ps", b_ya)])
            for c in range(8):
                P.pe(_mk("matmul", C.ps[b_ga][:], lhsT=ga_s[:, c, mm * 128:(mm + 1) * 128], rhs=hT[:, c, :],
                                             start=(c == 0), stop=(c == 7)),
                     reads=hTk + [gak], writes=[("ps", b_ga)])
            for cc in range(4):
                P.pe(_mk("matmul", C.ps[b_yb][:], lhsT=wb_s[:, cc, m * 128:(m + 1) * 128], rhs=o_bT[:, cc, :],
                                               start=(cc == 0), stop=(cc == 3)),
                     reads=obk + [wbk], writes=[("ps", b_yb)])
            for c in range(8):
                P.pe(_mk("matmul", C.ps[b_gb][:], lhsT=gb_s[:, c, mm * 128:(mm + 1) * 128], rhs=hT[:, c, :],
                                             start=(c == 0), stop=(c == 7)),
                     reads=hTk + [gbk], writes=[("ps", b_gb)])
            P.act(_mk("activation", out=sg[par][0][:], in_=C.ps[b_ga][:], func=AF.Sigmoid),
                  reads=[("ps", b_ga)], writes=[("sg", par, 0)])
            P.act(_mk("activation", out=sg[par][1][:], in_=C.ps[b_gb][:], func=AF.Sigmoid),
                  reads=[("ps", b_gb)], writes=[("sg", par, 1)])
            P.dve(_mk("tensor_tensor", out=t1[:], in0=C.ps[b_ya][:], in1=sg[par][0][:], op=ALU.mult),
                  reads=[("ps", b_ya), ("sg", par, 0)], writes=[t1k])
            P.dve(_mk("tensor_tensor", out=t2[:], in0=C.ps[b_yb][:], in1=sg[par][1][:], op=ALU.mult),
                  reads=[("ps", b_yb), ("sg", par, 1)], writes=[t2k])
            P.pool(_mk("tensor_tensor", out=yT[:, m, :], in0=t1[:], in1=t2[:], op=ALU.add),
                   reads=[t1k, t2k], writes=[("yT", m)])
        yTk = [("yT", m) for m in range(8)]
        xr, xrk = C.hf, "hf"
        stage(blk, 10)
        for t in range(4):
            T = blk * 4 + t
            for half in range(2):
                wo_s, wok = slab(blk, 10 + half)
                pb = (t * 2 + half) % 4
                for m in range(8):
                    P.pe(_mk("matmul", C.ps[pb][:], lhsT=yT[:, m, t * 128:(t + 1) * 128], rhs=wo_s[:, m, :],
                                                     start=(m == 0), stop=(m == 7)),
                         reads=yTk + [wok], writes=[("ps", pb)])
                P.dve(_mk("tensor_tensor", out=xr[:, half * 512:(half + 1) * 512], in0=C.ps[pb][:],
                                                           in1=gate1[:, half * 512:(half + 1) * 512], op=ALU.mult),
                      reads=[("ps", pb), "mod1"], writes=[xrk])
            xb = T % NXT
            P.dve(_mk("scalar_tensor_tensor", out=xr[:], in0=xt[xb][:], scalar=float(ALPHA), in1=xr[:],
                                                          op0=ALU.mult, op1=ALU.add),
                  reads=[("xt", xb), xrk], writes=[xrk])
            emit_ln_tile(C, xr[:], xrk, ln1g[:], ln1b[:], ["ln1g", "ln1b"], xo[:], "xo")
            P.dma("sp", d["x_out"][T * 128:(T + 1) * 128, :], xo[:], reads=["xo"], writes=["x_out"], slot="x_out")
        if STOP <= 6:
            break


def emit_moe(C, d):
    P = C.P
    nc = C.nc
    C.d = d
    NE = int(os.environ.get("MOE_NE", str(NEXP)))
    X = _sb(C, "X", [128, NT, D], F32)
    h2T = _sb(C, "h2T", [128, 8, TOK], BF16)
    mod = _sb(C, "mod2", [128, 3072], F32)
    sh2 = mod[:, 0:1024]
    sc2p = mod[:, 1024:2048]
    gate2 = mod[:, 2048:3072]
    G = _sb(C, "G", [128, NT, NEXP], F32)
    ln2g = _sb(C, "ln2g", [128, D], F32)
    ln2b = _sb(C, "ln2b", [128, D], F32)
    rb = _sb(C, "rb", [128, NEXP], F32)
    rw = _sb(C, "rw", [128, 8, NEXP], F32)
    h32T = _sb(C, "h32T", [128, 8, 128], F32)
    wgb = [_sb(C, "wgb%d" % i, [128, 8, 256], BF16) for i in range(2)]
    wub = [_sb(C, "wub%d" % i, [128, 8, 256], BF16) for i in range(2)]
    wdst = _sb(C, "wdst", [128, 2, D], F32)
    wdb = [_sb(C, "wdb%d" % i, [128, 2, D], BF16) for i in range(2)]
    sgt = [_sb(C, "sgt%d" % i, [128, 512], BF16) for i in range(2)]
    hbuf = [_sb(C, "hbuf%d" % i, [128, 2, 512], BF16) for i in range(2)]
    xo = [_sb(C, "xo%d" % i, [128, D], F32) for i in range(2)]
    r_sc = _sb(C, "r_sc", [128, NEXP], F32)
    r_sel = _sb(C, "r_sel", [128, NEXP], F32)
    r_eq = _sb(C, "r_eq", [128, NEXP], F32)
    r_sel2 = _sb(C, "r_sel2", [128, NEXP], F32)
    r_m1 = _sb(C, "r_m1", [128, 8], F32)
    r_m2 = _sb(C, "r_m2", [128, 8], F32)
    r_g8 = _sb(C, "r_g8", [128, 8], F32)
    r_gm = _sb(C, "r_gm", [128, 8], F32)
    r_e8 = _sb(C, "r_e8", [128, 8], F32)
    r_ss = _sb(C, "r_ss", [128, 1], F32)

    P.dma("sp", ln2g[:], d["ln2g"], writes=["ln2g"], slot=C.g)
    P.dma("sp", ln2b[:], d["ln2b"], writes=["ln2b"], slot=C.g)
    P.dma("sp", rb[:], d["rb"], writes=["rb"], slot=C.g)
    P.dma("sp", rw[:], d["rw"].rearrange("(p c) e -> p c e", c=8), writes=["rw"], slot=C.g)
    xin = d["x_in"].rearrange("(t p) f -> p t f", p=128)
    for i in range(4):
        P.dma("sp", X[:, i * 4:(i + 1) * 4, :], xin[:, i * 4:(i + 1) * 4, :],
              writes=[("X", T) for T in range(i * 4, i * 4 + 4)], slot=C.g)
    mw = [h2T[:, 0:8, :].rearrange("p c t -> p (c t)")[:, b * 8192:(b + 1) * 8192].bitcast(F32)
          .rearrange("p (c n) -> p c n", c=8) for b in range(2)]
    h2k = [("h2T", T) for T in range(NT)]
    emit_mod(C, 3072, 6, mod, "mod2", mw, [h2k, h2k])
    P.dve(_mk("tensor_scalar_add", out=sc2p, in0=sc2p, scalar1=1.0), reads=["mod2"], writes=["mod2"])

    def srcs(e):
        if e < NEXP:
            return d["wg"][e], d["wu"][e], d["wd"][e]
        return d["wsg"], d["wsu"], d["wsd"]

    def load_gu(e):
        bb = e % 2
        sg_, su_, _ = srcs(e)
        P.dma("pool", wgb[bb][:], sg_.rearrange("(p c) f -> p c f", c=8), writes=[("wgb", bb)], slot=("wgb", bb))
        P.dma("pool", wub[bb][:], su_.rearrange("(p c) f -> p c f", c=8), writes=[("wub", bb)], slot=("wub", bb))

    def load_d(e):
        bb = e % 2
        sd_ = srcs(e)[2]
        P.dma("sp", wdst[:], sd_.rearrange("(c p) n -> p c n", p=128), writes=["wdst"], slot="wdst")
        P.pool(_mk("tensor_tensor", out=wdb[bb][:], in0=wdst[:], in1=gate2.unsqueeze(1).to_broadcast([128, 2, D]),
                   op=ALU.mult), reads=["wdst", "mod2"], writes=[("wdb", bb)])

    hf = C.hf
    for T in range(NT):
        P.dve(_mk("tensor_tensor", out=hf[:], in0=X[:, T, :], in1=sc2p, op=ALU.mult), reads=[("X", T), "mod2"], writes=["hf"])
        P.pool(_mk("tensor_tensor", out=hf[:], in0=hf[:], in1=sh2, op=ALU.add), reads=["hf", "mod2"], writes=["hf"])
        P.act(_mk("mul", out=X[:, T, :], in_=X[:, T, :], mul=float(ALPHA)), reads=[("X", T)], writes=[("X", T)])
        hv = hf[:].rearrange("t (p c) -> t c p", c=8)
        pa, pb = 2 * (T % 2), 2 * (T % 2) + 1
        for c in range(8):
            bank = pa if c < 4 else pb
            P.pe(_mk("transpose", out=C.ps[bank][:, (c % 4) * 128:(c % 4 + 1) * 128], in_=hv[:, c, :], identity=C.ident_f[:]),
                 reads=["hf", "ident_f"], writes=[("ps", bank)])
        P.act(_mk("copy", out=h32T[:, 0:4, :], in_=C.ps[pa][:].rearrange("p (c k) -> p c k", c=4)),
              reads=[("ps", pa)], writes=["h32a"])
        P.act(_mk("copy", out=h32T[:, 4:8, :], in_=C.ps[pb][:].rearrange("p (c k) -> p c k", c=4)),
              reads=[("ps", pb)], writes=["h32b"])
        P.pool(_mk("tensor_copy", out=h2T[:, :, T * 128:(T + 1) * 128], in_=h32T[:]), reads=["h32a", "h32b"], writes=[("h2T", T)])
        rbk = 4 + T % 2
        for c in range(8):
            P.pe(_mk("matmul", C.ps[rbk][:, 0:NEXP], lhsT=h32T[:, c, :], rhs=rw[:, c, :], start=(c == 0), stop=(c == 7)),
                 reads=["h32a", "h32b", "rw"], writes=[("ps", rbk)])
        P.act(_mk("activation", out=r_sc[:], in_=C.ps[rbk][:, 0:NEXP], func=AF.Sigmoid), reads=[("ps", rbk)], writes=["r_sc"])
        P.dve(_mk("tensor_tensor", out=r_sel[:], in0=r_sc[:], in1=rb[:], op=ALU.add), reads=["r_sc", "rb"], writes=["r_sel"])
        sel3 = r_sel[:].rearrange("p (g k) -> p g k", k=8)
        P.dve(_mk("tensor_reduce", out=r_m1[:], in_=sel3, axis=AX.X, op=ALU.max), reads=["r_sel"], writes=["r_m1"])
        P.dve(_mk("tensor_tensor", out=r_eq[:].rearrange("p (g k) -> p g k", k=8), in0=sel3,
                  in1=r_m1[:].unsqueeze(2).to_broadcast([128, 8, 8]), op=ALU.is_equal), reads=["r_sel", "r_m1"], writes=["r_eq"])
        P.dve(_mk("scalar_tensor_tensor", out=r_sel2[:], in0=r_eq[:], scalar=-1.0e9, in1=r_sel[:], op0=ALU.mult, op1=ALU.add),
              reads=["r_eq", "r_sel"], writes=["r_sel2"])
        P.dve(_mk("tensor_reduce", out=r_m2[:], in_=r_sel2[:].rearrange("p (g k) -> p g k", k=8), axis=AX.X, op=ALU.max),
              reads=["r_sel2"], writes=["r_m2"])
        P.dve(_mk("tensor_tensor", out=r_m1[:], in0=r_m1[:], in1=r_m2[:], op=ALU.add), reads=["r_m1", "r_m2"], writes=["r_m1"])
        P.dve(_mk("max", out=r_g8[:], in_=r_m1[:]), reads=["r_m1"], writes=["r_g8"])
        P.dve(_mk("tensor_scalar", out=r_gm[:], in0=r_m1[:], scalar1=r_g8[:, 3:4], scalar2=None, op0=ALU.is_ge),
              reads=["r_m1", "r_g8"], writes=["r_gm"])
        P.dve(_mk("tensor_scalar_add", out=r_sel2[:], in0=r_sel[:], scalar1=2.0), reads=["r_sel"], writes=["r_sel2"])
        P.dve(_mk("tensor_tensor", out=r_eq[:].rearrange("p (g k) -> p g k", k=8),
                  in0=r_sel2[:].rearrange("p (g k) -> p g k", k=8),
                  in1=r_gm[:].unsqueeze(2).to_broadcast([128, 8, 8]), op=ALU.mult), reads=["r_sel2", "r_gm"], writes=["r_eq"])
        P.dve(_mk("max", out=r_e8[:], in_=r_eq[:]), reads=["r_eq"], writes=["r_e8"])
        P.dve(_mk("tensor_scalar", out=r_sel2[:], in0=r_eq[:], scalar1=r_e8[:, 7:8], scalar2=None, op0=ALU.is_ge),
              reads=["r_eq", "r_e8"], writes=["r_sel2"])
        P.dve(_mk("tensor_tensor", out=r_sel[:], in0=r_sc[:], in1=r_sel2[:], op=ALU.mult), reads=["r_sc", "r_sel2"], writes=["r_sel"])
        P.dve(_mk("tensor_reduce", out=r_ss[:], in_=r_sel[:], axis=AX.X, op=ALU.add), reads=["r_sel"], writes=["r_ss"])
        P.dve(_mk("reciprocal", out=r_ss[:], in_=r_ss[:]), reads=["r_ss"], writes=["r_ss"])
        P.dve(_mk("tensor_scalar", out=G[:, T, :], in0=r_sel[:], scalar1=r_ss[:, 0:1], scalar2=2.5, op0=ALU.mult, op1=ALU.mult),
              reads=["r_sel", "r_ss"], writes=[("G", T)])

    elist = list(range(NE)) + [NEXP]
    steps = [(ei, blk) for ei in range(len(elist)) for blk in range(NBLK)]
    cnt = [0]

    def GU(ei, blk):
        e = elist[ei]
        b = e % 2
        hb_ = hbuf[blk % 2]
        hk = ("hbuf", blk % 2)
        toks = slice(blk * 512, (blk + 1) * 512)
        tk = [("h2T", T) for T in range(blk * 4, blk * 4 + 4)]
        for ff in range(2):
            par = cnt[0] % 2
            cnt[0] += 1
            bg, bu = par, 2 + par
            for c in range(8):
                P.pe(_mk("matmul", C.ps[bg][:], lhsT=wgb[b][:, c, ff * 128:(ff + 1) * 128], rhs=h2T[:, c, toks],
                         start=(c == 0), stop=(c == 7)), reads=tk + [("wgb", b)], writes=[("ps", bg)])
            for c in range(8):
                P.pe(_mk("matmul", C.ps[bu][:], lhsT=wub[b][:, c, ff * 128:(ff + 1) * 128], rhs=h2T[:, c, toks],
                         start=(c == 0), stop=(c == 7)), reads=tk + [("wub", b)], writes=[("ps", bu)])
            P.act(_mk("activation", out=sgt[par][:], in_=C.ps[bg][:], func=AF.Silu), reads=[("ps", bg)], writes=[("sgt", par)])
            P.dve(_mk("tensor_tensor", out=hb_[:, ff, :], in0=C.ps[bu][:], in1=sgt[par][:], op=ALU.mult),
                  reads=[("ps", bu), ("sgt", par)], writes=[(hk, ff)])
        if blk == NBLK - 1 and ei + 2 < len(elist):
            load_gu(elist[ei + 2])

    def Y(ei, blk):
        e = elist[ei]
        b = e % 2
        hb_ = hbuf[blk % 2]
        hk = ("hbuf", blk % 2)
        for t in range(4):
            T = blk * 4 + t
            for half in range(2):
                by = 4 + (t * 2 + half) % 4
                for ff in range(2):
                    P.pe(_mk("matmul", C.ps[by][:], lhsT=hb_[:, ff, t * 128:(t + 1) * 128],
                             rhs=wdb[b][:, ff, half * 512:(half + 1) * 512], start=(ff == 0), stop=(ff == 1)),
                         reads=[(hk, 0), (hk, 1), ("wdb", b)], writes=[("ps", by)])
                xs = X[:, T, half * 512:(half + 1) * 512]
                scal = G[:, T, e:e + 1] if e < NEXP else 1.0
                P.dve(_mk("scalar_tensor_tensor", out=xs, in0=C.ps[by][:], scalar=scal, in1=xs, op0=ALU.mult, op1=ALU.add),
                      reads=[("ps", by), ("G", T), ("X", T)], writes=[("X", T)])
        if blk == NBLK - 1 and ei + 2 < len(elist):
            load_d(elist[ei + 2])

    for ei in range(min(2, len(elist))):
        load_gu(elist[ei])
        load_d(elist[ei])
    GU(*steps[0])
    for k in range(len(steps)):
        if k + 1 < len(steps):
            GU(*steps[k + 1])
        Y(*steps[k])
    for T in range(NT):
        o = xo[T % 2]
        ok = ("xo", T % 2)
        emit_ln_tile(C, X[:, T, :], ("X", T), ln2g[:], ln2b[:], ["ln2g", "ln2b"], o[:], ok)
        P.dma("sp", d["x_out"][T * 128:(T + 1) * 128, :], o[:], reads=[ok], writes=[("x_out", T)], slot=("x_out", T % 2))


def rope_tables():
    t = np.arange(SEQ)
    rows = SEQ // 64
    row = (t // 64 - rows // 2).astype(np.float32)
    col = (t % 64 - 32).astype(np.float32)
    inv = (np.float32(10000.0) ** (-np.arange(16, dtype=np.float32) / np.float32(16))).astype(np.float32)
    ang = np.concatenate([row[:, None] * inv, col[:, None] * inv], -1).astype(np.float32)
    return np.cos(ang).astype(np.float32), np.sin(ang).astype(np.float32)


def tile_major(a):
    return np.ascontiguousarray(a.reshape(NT, 128, -1).transpose(1, 0, 2))


def rep128(v):
    return np.ascontiguousarray(np.broadcast_to(np.asarray(v, np.float32).reshape(1, -1), (128, v.size)))


PAIR_GROUPS = [[0, 1], [2, 3], [4, 5], [6, 7]]


def build_fused(nc, nlayers=DEPTH, groups=PAIR_GROUPS):
    C = Ctx()
    C.nc = nc
    C.P = P = Prog(nc)
    L = nlayers
    I = {}
    I["x_in"] = _din(nc, "x_in", [TOK, D])
    I["cT"] = _din(nc, "cT", [128, 8])
    I["w_ada"] = _din(nc, "w_ada", [L, D, 6 * D])
    I["b_ada"] = _din(nc, "b_ada", [L, 1, 6 * D])
    I["w_in"] = _din(nc, "w_in", [L, D, 3840])
    I["qg"] = _din(nc, "qg", [L, 128, 512])
    I["kg"] = _din(nc, "kg", [L, 128, 128])
    I["cos"] = _din(nc, "cos", [128, NT, 32])
    I["sin"] = _din(nc, "sin", [128, NT, 32])
    I["lng"] = _din(nc, "lng", [L, 128, 512])
    I["lnb"] = _din(nc, "lnb", [L, 128, 512])
    I["w_sp"] = _din(nc, "w_sp", [L, 8, 128, 128])
    I["bsT"] = _din(nc, "bsT", [L, 128, 4, 128])
    I["wa"] = _din(nc, "wa", [L, 512, D])
    I["wb"] = _din(nc, "wb", [L, 512, D])
    I["wo"] = _din(nc, "wo", [L, D, D])
    I["ln1g"] = _din(nc, "ln1g", [L, 128, D])
    I["ln1b"] = _din(nc, "ln1b", [L, 128, D])
    I["rw"] = _din(nc, "rw", [L, D, NEXP])
    I["rb"] = _din(nc, "rb", [L, 128, NEXP])
    I["wg"] = _din(nc, "wg", [L, NEXP, D, 256])
    I["wu"] = _din(nc, "wu", [L, NEXP, D, 256])
    I["wd"] = _din(nc, "wd", [L, NEXP, 256, D])
    I["wsg"] = _din(nc, "wsg", [L, D, 256])
    I["wsu"] = _din(nc, "wsu", [L, D, 256])
    I["wsd"] = _din(nc, "wsd", [L, 256, D])
    I["ln2g"] = _din(nc, "ln2g", [L, 128, D])
    I["ln2b"] = _din(nc, "ln2b", [L, 128, D])
    x_out = _dout(nc, "x_out", [TOK, D])

    def scratch(name, shape, dt):
        return nc.dram_tensor(name, list(shape), dt, kind="Internal").ap()

    xa = scratch("xa", [TOK, D], F32)
    xb = scratch("xb", [TOK, D], F32)
    kT_loc = [scratch("kT_loc%d" % l, [128, TOK], BF16) for l in range(L)]
    v_loc = [scratch("v_loc%d" % l, [TOK, 130], BF16) for l in range(L)]
    kT_all = [scratch("kT_all%d" % l, [256, TOK], BF16) for l in range(L)]
    v_all = [scratch("v_all%d" % l, [2 * TOK, 130], BF16) for l in range(L)]
    phase = [0]

    def newphase():
        phase[0] += 1
        C.g = "G:init%d" % phase[0]
        C.gp = "G:initp%d" % phase[0]
        C.arena_off = C.arena_base

    with contextlib.ExitStack() as stack:
        C.arena = stack.enter_context(nc.sbuf_tensor("arena", [128, ARENA_ELEMS], BF16))
        C.arena_off = 0
        C.ps = [stack.enter_context(nc.psum_tensor("ps%d" % i, [128, 512], F32)) for i in range(8)]
        emit_consts(C)
        alloc_common(C)
        alloc_normrope(C)
        C.arena_base = C.arena_off
        xcur = I["x_in"]
        for l in range(L):
            common = {"cT": I["cT"], "w_ada": I["w_ada"][l], "b_ada": I["b_ada"][l], "w_in": I["w_in"][l],
                      "cos": I["cos"], "sin": I["sin"]}
            newphase()
            d = dict(common)
            d.update({"x_in": xcur, "kg": I["kg"][l], "kT_out": kT_loc[l], "v_out": v_loc[l]})
            emit_kv(C, d)
            P.barrier()
            P.add("pool", _mk("collective_compute", "AllGather", ALU.bypass, replica_groups=groups,
                              ins=[kT_loc[l]], outs=[kT_all[l]]), dma="cc", inc=1)
            P.add("pool", _mk("collective_compute", "AllGather", ALU.bypass, replica_groups=groups,
                              ins=[v_loc[l]], outs=[v_all[l]]), dma="cc", inc=1)
            P.barrier()
            newphase()
            d = dict(common)
            d.update({"x_in": xcur, "x_out": xa, "qg": I["qg"][l], "kT_all": kT_all[l], "v_full": v_all[l],
                      "lng": I["lng"][l], "lnb": I["lnb"][l], "w_sp": I["w_sp"][l], "bsT": I["bsT"][l],
                      "wa": I["wa"][l], "wb": I["wb"][l], "wo": I["wo"][l], "ln1g": I["ln1g"][l], "ln1b": I["ln1b"][l]})
            emit_mix(C, d)
            P.barrier()
            newphase()
            xnext = x_out if l == L - 1 else xb
            d = dict(common)
            d.update({"x_in": xa, "x_out": xnext, "rw": I["rw"][l], "rb": I["rb"][l], "wg": I["wg"][l], "wu": I["wu"][l],
                      "wd": I["wd"][l], "wsg": I["wsg"][l], "wsu": I["wsu"][l], "wsd": I["wsd"][l],
                      "ln2g": I["ln2g"][l], "ln2b": I["ln2b"][l]})
            emit_moe(C, d)
            P.barrier()
            xcur = xb
        P.emit(final_all=True)
    return nc


def host_inputs(inp, nlayers=DEPTH, l0=0, x_cores=None):
    L = nlayers
    cosf, sinf = rope_tables()
    f32 = np.float32

    inp = dict(inp)
    for k in list(inp.keys()):
        if k not in ("x", "c"):
            inp[k] = np.asarray(inp[k])[l0:l0 + L]

    def rep(v):
        v = np.asarray(v, f32)[:L]
        return np.ascontiguousarray(np.broadcast_to(v[:, None, :], (L, 128, v.shape[1])))

    bs = np.asarray(inp["b_spatial"], f32)[:L]
    idx = (2 * np.arange(4)[None, :] + (np.arange(128) // 64)[:, None])
    bsT = np.ascontiguousarray(bs[:, idx, :])
    shared = {
        "w_ada": np.ascontiguousarray(inp["w_ada"][:L]),
        "b_ada": np.ascontiguousarray(inp["b_ada"][:L].reshape(L, 1, -1)),
        "w_in": np.ascontiguousarray(inp["w_in"][:L]),
        "qg": rep(np.tile(inp["q_scale"], (1, 8))),
        "kg": rep(np.tile(inp["k_scale"], (1, 2))),
        "lng": rep(inp["sgu_ln_g"]), "lnb": rep(inp["sgu_ln_b"]),
        "w_sp": np.ascontiguousarray(inp["w_spatial"][:L]),
        "bsT": bsT,
        "wa": np.ascontiguousarray(inp["w_branch_a"][:L]), "wb": np.ascontiguousarray(inp["w_branch_b"][:L]),
        "wo": np.ascontiguousarray(inp["w_out"][:L]),
        "ln1g": rep(inp["ln1_g"]), "ln1b": rep(inp["ln1_b"]),
        "rw": np.ascontiguousarray(inp["router_w"][:L]), "rb": rep(inp["router_bias"]),
        "wg": np.ascontiguousarray(inp["w_gate"][:L]), "wu": np.ascontiguousarray(inp["w_up"][:L]),
        "wd": np.ascontiguousarray(inp["w_down"][:L]),
        "wsg": np.ascontiguousarray(inp["ws_gate"][:L]), "wsu": np.ascontiguousarray(inp["ws_up"][:L]),
        "wsd": np.ascontiguousarray(inp["ws_down"][:L]),
        "ln2g": rep(inp["ln2_g"]), "ln2b": rep(inp["ln2_b"]),
    }
    maps = []
    for r in range(8):
        b, half = r // 2, r % 2
        sl = slice(half * TOK, (half + 1) * TOK)
        m = dict(shared)
        m["x_in"] = np.ascontiguousarray(inp["x"][b, sl]) if x_cores is None else x_cores[r]
        m["cT"] = np.ascontiguousarray(np.asarray(inp["c"], f32)[b].reshape(8, 128).T)
        m["cos"] = tile_major(cosf[sl])
        m["sin"] = tile_major(sinf[sl])
        maps.append(m)
    return maps


_NC = {}
LAYERS_PER_LAUNCH = 2


def kernel(**inp):
    inp = {k: np.asarray(v) for k, v in inp.items()}
    LPL = LAYERS_PER_LAUNCH
    if LPL not in _NC:
        nc = bass.Bass("TRN2", target_bir_lowering=False)
        build_fused(nc, nlayers=LPL)
        _NC[LPL] = nc
    x_cores = None
    for l0 in range(0, DEPTH, LPL):
        maps = host_inputs(inp, nlayers=LPL, l0=l0, x_cores=x_cores)
        res = run_bass_kernel_spmd(_NC[LPL], maps, core_ids=list(range(8)))
        x_cores = [np.ascontiguousarray(res.results[r]["x_out"]) for r in range(8)]
    out = np.zeros((4, SEQ, D), np.float32)
    for r in range(8):
        out[r // 2, (r % 2) * TOK:(r % 2 + 1) * TOK] = x_cores[r]
    return out
```
